# Optimizing a Trainium2 kernel written in Bass

```python
import jax, jax.numpy as jnp
from jax import lax
import numpy as np

D_MODEL = 2048
BATCH = 1
SEQ = 16384
DEPTH = 1

CHUNK = 64
EPS = 1e-6
MIX_WIDTH = D_MODEL

SSD_HEAD_DIM = 64
SSD_INNER = MIX_WIDTH // 2
SSD_HEADS = SSD_INNER // SSD_HEAD_DIM
SSD_GROUPS = 2
SSD_STATE = 128
SSD_CONV = 4
SSD_CONV_DIM = SSD_INNER + 2 * SSD_GROUPS * SSD_STATE

FOX_HEAD_DIM = 64
FOX_INNER = MIX_WIDTH - SSD_INNER
FOX_HEADS = FOX_INNER // FOX_HEAD_DIM
Q_BLOCK = 128

PEER_HEADS = 8
PEER_N_KEYS = 128
PEER_N_EXPERTS = PEER_N_KEYS * PEER_N_KEYS
PEER_KEY_DIM = 256
PEER_TOPK = 16
PEER_TOKEN_BLOCK = 128

IN_SPLITS = (SSD_INNER, SSD_CONV_DIM, SSD_HEADS, FOX_INNER, FOX_INNER, FOX_INNER, FOX_HEADS)
IN_PROJ_DIM = sum(IN_SPLITS)

kernel_name = "hymba_ssd_fox_peer_block"


def rmsnorm(x, w):
    xf = x.astype(jnp.float32)
    xf = xf * lax.rsqrt(jnp.mean(xf * xf, axis=-1, keepdims=True) + EPS)
    return (xf * w.astype(jnp.float32)).astype(x.dtype)


def ssd_chunked(xs, dt, A, Bm, Cm):
    b, l = xs.shape[:2]
    nc = l // CHUNK
    R = SSD_HEADS // SSD_GROUPS
    x = xs.astype(jnp.float32).reshape(b, nc, CHUNK, SSD_GROUPS, R, SSD_HEAD_DIM)
    dtc = dt.reshape(b, nc, CHUNK, SSD_GROUPS, R)
    Bc = Bm.astype(jnp.float32).reshape(b, nc, CHUNK, SSD_GROUPS, SSD_STATE)
    Cc = Cm.astype(jnp.float32).reshape(b, nc, CHUNK, SSD_GROUPS, SSD_STATE)
    a = dtc * A.reshape(SSD_GROUPS, R)
    a_cum = jnp.cumsum(a, axis=2)
    xdt = x * dtc[..., None]
    seg = a_cum[:, :, :, None] - a_cum[:, :, None]
    tri = jnp.tril(jnp.ones((CHUNK, CHUNK), dtype=bool))[None, None, :, :, None, None]
    L = jnp.exp(jnp.where(tri, seg, -jnp.inf))
    cb = jnp.einsum('bctgn,bcsgn->bctsg', Cc, Bc)
    y_diag = jnp.einsum('bctsg,bctsgr,bcsgrp->bctgrp', cb, L, xdt)
    decay_states = jnp.exp(a_cum[:, :, -1:] - a_cum)
    states = jnp.einsum('bcsgn,bcsgr,bcsgrp->bcgrpn', Bc, decay_states, xdt)
    chunk_decay = jnp.exp(a_cum[:, :, -1])

    def step(h, inp):
        s_c, d_c = inp
        return h * d_c[..., None, None] + s_c, h

    h0 = jnp.zeros((b, SSD_GROUPS, R, SSD_HEAD_DIM, SSD_STATE), jnp.float32)
    _, prev = lax.scan(step, h0, (jnp.moveaxis(states, 1, 0), jnp.moveaxis(chunk_decay, 1, 0)))
    prev = jnp.moveaxis(prev, 0, 1)
    y_off = jnp.einsum('bctgn,bcgrpn,bctgr->bctgrp', Cc, prev, jnp.exp(a_cum))
    return (y_diag + y_off).reshape(b, l, SSD_HEADS, SSD_HEAD_DIM)


def ssd_group(z, xbc, dt_raw, conv_w, conv_b, dt_bias, a_log, d_skip, ssd_norm_w):
    b, l = z.shape[:2]
    xbc = lax.conv_general_dilated(
        xbc, conv_w[:, None, :], window_strides=(1,), padding=[(SSD_CONV - 1, 0)],
        dimension_numbers=('NWC', 'WIO', 'NWC'), feature_group_count=SSD_CONV_DIM) + conv_b
    xbc = jax.nn.silu(xbc)
    xs, Bm, Cm = jnp.split(xbc, [SSD_INNER, SSD_INNER + SSD_GROUPS * SSD_STATE], axis=-1)
    xs = xs.reshape(b, l, SSD_HEADS, SSD_HEAD_DIM)
    Bm = Bm.reshape(b, l, SSD_GROUPS, SSD_STATE)
    Cm = Cm.reshape(b, l, SSD_GROUPS, SSD_STATE)
    dt = jax.nn.softplus(dt_raw.astype(jnp.float32) + dt_bias.astype(jnp.float32))
    A = -jnp.exp(a_log.astype(jnp.float32))
    y = ssd_chunked(xs, dt, A, Bm, Cm)
    y = y + d_skip.astype(jnp.float32)[:, None] * xs.astype(jnp.float32)
    y = y.reshape(b, l, SSD_INNER) * jax.nn.silu(z.astype(jnp.float32))
    y = y.reshape(b, l, SSD_GROUPS, SSD_INNER // SSD_GROUPS)
    y = y * lax.rsqrt(jnp.mean(y * y, axis=-1, keepdims=True) + EPS)
    y = y.reshape(b, l, SSD_INNER) * ssd_norm_w.astype(jnp.float32)
    return y.astype(z.dtype)


def fox_group(q, k, v, f_raw, fox_f_bias):
    b, l = q.shape[:2]
    q = q.reshape(b, l, FOX_HEADS, FOX_HEAD_DIM) * (FOX_HEAD_DIM ** -0.5)
    k = k.reshape(b, l, FOX_HEADS, FOX_HEAD_DIM)
    v = v.reshape(b, l, FOX_HEADS, FOX_HEAD_DIM)
    log_f = jax.nn.log_sigmoid(f_raw.astype(jnp.float32) + fox_f_bias.astype(jnp.float32))
    cum = jnp.transpose(jnp.cumsum(log_f, axis=1), (0, 2, 1))
    outs = []
    for i in range(l // Q_BLOCK):
        qs, qe = i * Q_BLOCK, (i + 1) * Q_BLOCK
        s = jnp.einsum('bqhd,bkhd->bhqk', q[:, qs:qe], k[:, :qe]).astype(jnp.float32)
        s = s + cum[:, :, qs:qe, None] - cum[:, :, None, :qe]
        mask = (qs + jnp.arange(Q_BLOCK))[:, None] >= jnp.arange(qe)[None, :]
        p = jax.nn.softmax(jnp.where(mask, s, -jnp.inf), axis=-1)
        outs.append(jnp.einsum('bhqk,bkhd->bqhd', p.astype(v.dtype), v[:, :qe]))
    return jnp.concatenate(outs, axis=1).reshape(b, l, FOX_INNER)


def peer(h, wq, k1, k2, u, v):
    b, l, d = h.shape
    t = h.reshape(-1, d)
    T = t.shape[0]
    q = (t @ wq).reshape(T, PEER_HEADS, 2, PEER_KEY_DIM // 2)
    s1 = jnp.einsum('thd,hnd->thn', q[:, :, 0], k1).astype(jnp.float32)
    s2 = jnp.einsum('thd,hnd->thn', q[:, :, 1], k2).astype(jnp.float32)
    v1, i1 = lax.top_k(s1, PEER_TOPK)
    v2, i2 = lax.top_k(s2, PEER_TOPK)
    cand = (v1[..., :, None] + v2[..., None, :]).reshape(T, PEER_HEADS, PEER_TOPK * PEER_TOPK)
    cand_idx = (i1[..., :, None] * PEER_N_KEYS + i2[..., None, :]).reshape(T, PEER_HEADS, PEER_TOPK * PEER_TOPK)
    sc, pos = lax.top_k(cand, PEER_TOPK)
    idx = jnp.take_along_axis(cand_idx, pos, axis=-1)
    g = jax.nn.softmax(sc, axis=-1).astype(t.dtype)

    def block(args):
        tb, ib, gb = args
        act = jax.nn.gelu(jnp.einsum('thkd,td->thk', u[ib], tb), approximate=False)
        return jnp.einsum('thk,thkd->td', gb * act, v[ib])

    nb = T // PEER_TOKEN_BLOCK
    out = lax.map(block, (t.reshape(nb, PEER_TOKEN_BLOCK, d),
                          idx.reshape(nb, PEER_TOKEN_BLOCK, PEER_HEADS, PEER_TOPK),
                          g.reshape(nb, PEER_TOKEN_BLOCK, PEER_HEADS, PEER_TOPK)))
    return out.reshape(b, l, d)


def setup_inputs(seed: int = 0) -> dict:
    key = jax.random.key(seed)
    ks = jax.random.split(key, 20)
    f32 = jnp.float32
    nrm = lambda k, shape: jax.random.normal(k, shape, f32)
    dt0 = jnp.exp(jax.random.uniform(ks[5], (SSD_HEADS,), f32, np.log(1e-3), np.log(1e-1)))
    return {
        "x": nrm(ks[0], (BATCH, SEQ, D_MODEL)),
        "ln1_w": 1.0 + 0.02 * nrm(ks[1], (D_MODEL,)),
        "w_in": nrm(ks[2], (D_MODEL, IN_PROJ_DIM)) * D_MODEL ** -0.5,
        "conv_w": nrm(ks[3], (SSD_CONV, SSD_CONV_DIM)) * SSD_CONV ** -0.5,
        "conv_b": 0.02 * nrm(ks[4], (SSD_CONV_DIM,)),
        "dt_bias": dt0 + jnp.log(-jnp.expm1(-dt0)),
        "a_log": jnp.log(jax.random.uniform(ks[6], (SSD_HEADS,), f32, 1.0, 16.0)),
        "d_skip": 1.0 + 0.1 * nrm(ks[7], (SSD_HEADS,)),
        "ssd_norm_w": 1.0 + 0.02 * nrm(ks[8], (SSD_INNER,)),
        "fox_f_bias": jnp.linspace(1.0, 6.0, FOX_HEADS, dtype=f32) + 0.1 * nrm(ks[9], (FOX_HEADS,)),
        "w_out": nrm(ks[10], (MIX_WIDTH, D_MODEL)) * MIX_WIDTH ** -0.5,
        "ln2_w": 1.0 + 0.02 * nrm(ks[11], (D_MODEL,)),
        "peer_wq": nrm(ks[12], (D_MODEL, PEER_HEADS * PEER_KEY_DIM)) * D_MODEL ** -0.5,
        "peer_k1": nrm(ks[13], (PEER_HEADS, PEER_N_KEYS, PEER_KEY_DIM // 2)) * (PEER_KEY_DIM // 2) ** -0.5,
        "peer_k2": nrm(ks[14], (PEER_HEADS, PEER_N_KEYS, PEER_KEY_DIM // 2)) * (PEER_KEY_DIM // 2) ** -0.5,
        "peer_u": nrm(ks[15], (PEER_N_EXPERTS, D_MODEL)) * D_MODEL ** -0.5,
        "peer_v": nrm(ks[16], (PEER_N_EXPERTS, D_MODEL)) * PEER_HEADS ** -0.5,
        "lnf_w": 1.0 + 0.02 * nrm(ks[17], (D_MODEL,)),
    }


def reference(x, ln1_w, w_in, conv_w, conv_b, dt_bias, a_log, d_skip, ssd_norm_w,
              fox_f_bias, w_out, ln2_w, peer_wq, peer_k1, peer_k2, peer_u, peer_v, lnf_w):
    split_at = [int(s) for s in np.cumsum(IN_SPLITS)[:-1]]
    for _ in range(DEPTH):
        h = rmsnorm(x, ln1_w)
        proj = h @ w_in
        z, xbc, dt_raw, q, k, v, f_raw = jnp.split(proj, split_at, axis=-1)
        y_ssd = ssd_group(z, xbc, dt_raw, conv_w, conv_b, dt_bias, a_log, d_skip, ssd_norm_w)
        y_fox = fox_group(q, k, v, f_raw, fox_f_bias)
        x = x + jnp.concatenate([y_ssd, y_fox], axis=-1) @ w_out
        x = x + peer(rmsnorm(x, ln2_w), peer_wq, peer_k1, peer_k2, peer_u, peer_v)
    return rmsnorm(x, lnf_w)
```

```python
from concourse.bass_utils import run_bass_kernel_spmd

import numpy as np
import concourse.bass as bass
import concourse.mybir as mybir
from contextlib import ExitStack

F32 = mybir.dt.float32
BF16 = mybir.dt.bfloat16
AF = mybir.ActivationFunctionType
ALU = mybir.AluOpType
AX = mybir.AxisListType


class Buf:
    __slots__ = ("w", "r", "name")

    def __init__(self, name=""):
        self.w = None
        self.r = []
        self.name = name


class Prog:
    ENG = ["sync", "scalar", "vector", "gpsimd", "tensor"]

    def __init__(self, nc, es, n_dma_sems=24):
        self.nc = nc
        self.es = es
        self.ops = {e: [] for e in self.ENG}
        self.sem = {e: es.enter_context(nc.semaphore("s_" + e)) for e in self.ENG}
        self.cnt = {e: 0 for e in self.ENG}
        self.dsem = [es.enter_context(nc.semaphore(f"dq{i}")) for i in range(n_dma_sems)]
        self.dcnt = [0] * n_dma_sems
        self.dnext = 0
        self.waited = {e: {} for e in self.ENG}
        self.nwaits = 0

    def _semof(self, key):
        return self.sem[key[1]] if key[0] == "e" else self.dsem[key[1]]

    def _issue(self, eng, fn, reads, writes, key, inc, extra_deps=()):
        deps = set(extra_deps)
        for b in reads:
            if b.w is not None:
                deps.add(b.w)
        for b in writes:
            if b.w is not None:
                deps.add(b.w)
            deps.update(b.r)
        waits = []
        best = {}
        for (k, v) in deps:
            if k == ("e", "tensor") and key == ("e", "tensor"):
                continue
            if best.get(k, 0) < v:
                best[k] = v
        for k, v in best.items():
            if self.waited[eng].get(k, 0) >= v:
                continue
            self.waited[eng][k] = v
            waits.append((self._semof(k), v))
        self.nwaits += len(waits)
        if key[0] == "e":
            self.cnt[eng] += inc
            val = self.cnt[eng]
        else:
            self.dcnt[key[1]] += inc
            val = self.dcnt[key[1]]
        self.ops[eng].append((waits, fn, (self._semof(key), inc)))
        tok = (key, val)
        for b in reads:
            b.r.append(tok)
        for b in writes:
            b.w = tok
            b.r = []
        return tok

    def op(self, eng, fn, reads=(), writes=()):
        return self._issue(eng, fn, list(reads), list(writes), ("e", eng), 1)

    def opk(self, eng, name, kw, reads=(), writes=()):
        return self.op(eng, lambda e: getattr(e, name)(**kw), reads, writes)

    def dma(self, eng, out, in_, reads=(), writes=(), **kw):
        i = self.dnext % len(self.dsem)
        self.dnext += 1
        key = ("d", i)
        extra = []
        if self.dcnt[i] > 0:
            extra.append((key, self.dcnt[i]))
        return self._issue(eng, lambda e: e.dma_start(out=out, in_=in_, **kw),
                           list(reads), list(writes), key, 16, extra)

    def mm(self, out, lhsT, rhs, start, stop, reads=(), writes=()):
        return self.op("tensor", lambda e: e.matmul(out, lhsT, rhs, start=start, stop=stop),
                       reads, writes)

    def act(self, out, in_, func, reads=(), writes=(), **kw):
        return self.op("scalar", lambda e: e.activation(out=out, in_=in_, func=func, **kw),
                       reads, writes)

    def finish(self, bufs=()):
        deps = []
        for i, c in enumerate(self.dcnt):
            if c > 0:
                deps.append((("d", i), c))
        for e in self.ENG:
            if e != "sync" and self.cnt[e] > 0:
                deps.append((("e", e), self.cnt[e]))
        waits = []
        for k, v in deps:
            if self.waited["sync"].get(k, 0) >= v:
                continue
            self.waited["sync"][k] = v
            waits.append((self._semof(k), v))
        self.ops["sync"].append((waits, None, None))

    def barrier(self):
        deps = []
        for i, c in enumerate(self.dcnt):
            if c > 0:
                deps.append((("d", i), c))
        for e in self.ENG:
            if self.cnt[e] > 0:
                deps.append((("e", e), self.cnt[e]))
        for e in self.ENG:
            waits = []
            for k, v in deps:
                if k == ("e", e) and e == "tensor":
                    continue
                if self.waited[e].get(k, 0) >= v:
                    continue
                self.waited[e][k] = v
                waits.append((self._semof(k), v))
            self.ops[e].append((waits, None, None))

    def emit(self):
        with self.nc.Block() as block:
            for e in self.ENG:
                ops = self.ops[e]

                def body(eng, ops=ops):
                    for (waits, fn, inc) in ops:
                        for (s, v) in waits:
                            eng.wait_ge(s, v)
                        if fn is None:
                            continue
                        ins = fn(eng)
                        ins.then_inc(inc[0], inc[1])

                getattr(block, e)(body)
        self.ops = {e: [] for e in self.ENG}


D = 2048
NCH = 16
FM = [("z", 0, 128), ("xs", 128, 128), ("B", 256, 128), ("C", 384, 128),
      ("q", 512, 128), ("k", 640, 128)]
TM0 = 768
NW = 900


def build_p1(T):
    NT = T // 512
    NB = T // 128
    nc = bass.Bass("TRN2", target_bir_lowering=False)
    xT = nc.dram_tensor("xT", [D, T], F32, kind="ExternalInput").ap()
    wf = nc.dram_tensor("wf", [D, NW], F32, kind="ExternalInput").ap()
    ln1 = nc.dram_tensor("ln1", [128, NCH], F32, kind="ExternalInput").ap()
    cwd = nc.dram_tensor("cw", [128, 12], F32, kind="ExternalInput").ap()
    cbd = nc.dram_tensor("cb", [128, 3], F32, kind="ExternalInput").ap()
    hpd = nc.dram_tensor("hp", [128, 8], F32, kind="ExternalInput").ap()
    yT = nc.dram_tensor("yT", [256, T], F32, kind="ExternalOutput").ap()
    xTv = xT.rearrange("(c p) t -> p c t", p=128)

    with ExitStack() as es:
        P = Prog(nc, es)

        def sb(name, shape, dt=F32):
            return es.enter_context(nc.sbuf_tensor(name, shape, dt))

        def pt(name, shape, dt=F32):
            return es.enter_context(nc.psum_tensor(name, shape, dt))

        V = lambda f, kw, r=(), w=(): P.opk("vector", f, kw, r, w)
        G = lambda f, kw, r=(), w=(): P.opk("gpsimd", f, kw, r, w)
        A = lambda f, kw, r=(), w=(): P.opk("scalar", f, kw, r, w)

        cB = Buf("const")
        U = sb("U", [128, 128])
        SL = sb("SL", [128, 128])
        ones = sb("ones", [128, 128])
        onesb = sb("onesb", [128, 128], BF16)
        idb = sb("idb", [128, 128], BF16)
        idf = sb("idf", [128, 128])
        G("memset", dict(ap=U[:], constant=1.0), w=[cB])
        G("affine_select", dict(out=U[:], in_=U[:], pattern=[[1, 128]], compare_op=ALU.is_ge,
                                    fill=0.0, base=0, channel_multiplier=-1), w=[cB])
        G("memset", dict(ap=SL[:], constant=1.0), w=[cB])
        G("affine_select", dict(out=SL[:], in_=SL[:], pattern=[[-1, 128]], compare_op=ALU.is_gt,
                                    fill=0.0, base=0, channel_multiplier=1), w=[cB])
        G("memset", dict(ap=ones[:], constant=1.0), w=[cB])
        G("memset", dict(ap=onesb[:], constant=1.0), w=[cB])
        G("memset", dict(ap=idf[:], constant=1.0), w=[cB])
        G("affine_select", dict(out=idf[:], in_=idf[:], pattern=[[1, 128]], compare_op=ALU.is_equal,
                                    fill=0.0, base=0, channel_multiplier=-1), w=[cB])
        G("tensor_copy", dict(out=idb[:], in_=idf[:]), r=[cB], w=[cB])

        ln1t = sb("ln1t", [128, NCH])
        cw = sb("cwt", [128, 12])
        cb = sb("cbt", [128, 3])
        hp = sb("hpt", [128, 8])
        P.dma("sync", ln1t[:], ln1, writes=[cB])
        P.dma("sync", cw[:], cwd, writes=[cB])
        P.dma("sync", cb[:], cbd, writes=[cB])
        P.dma("sync", hp[:], hpd, writes=[cB])
        hq = sb("hq", [128, 4])
        A("activation", dict(out=hq[:, 0:2], in_=hp[:, 2:4], func=AF.Exp), r=[cB], w=[cB])
        V("tensor_scalar", dict(out=hq[:, 0:2], in0=hq[:, 0:2], scalar1=-1.0, scalar2=None, op0=ALU.mult), r=[cB], w=[cB])
        V("tensor_scalar", dict(out=hq[:, 2:4], in0=hp[:, 6:8], scalar1=-1.0, scalar2=None, op0=ALU.mult), r=[cB], w=[cB])

        W = sb("W", [128, NCH, NW], BF16)
        wB = Buf("W")
        wst1 = sb("wst", [128, NW])
        wst = [wst1] * 2
        wstB = [Buf()] * 2
        for c in range(NCH):
            k = c % 2
            P.dma("sync", wst[k][:], wf[c * 128:(c + 1) * 128, :], writes=[wstB[k]])
            V("tensor_scalar", dict(out=W[:, c, :], in0=wst[k][:], scalar1=ln1t[:, c:c + 1],
                                                 scalar2=None, op0=ALU.mult), r=[wstB[k], cB], w=[wB])

        k2 = sb("k2", [128, T], BF16)
        kB_ = [Buf() for _ in range(NT)]
        Vaug = sb("Vaug", [128, NB, 2, 68], BF16)
        vB_ = [Buf() for _ in range(NT)]
        Ccol = sb("Ccol", [128, 2, NB])
        ccB = [Buf(), Buf()]
        carry = sb("carry", [128, 2])
        pre = [sb(f"pre{h}", [128, 5]) for h in range(2)]
        pad = [sb(f"pad{h}", [128, 4], BF16) for h in range(2)]
        padB = [Buf(), Buf()]
        hTf = [sb(f"hTf{h}", [128, 64]) for h in range(2)]
        hTb = [sb(f"hTb{h}", [128, 64], BF16) for h in range(2)]
        hfB = [Buf(), Buf()]
        hbB = [Buf(), Buf()]
        xbc = [sb(f"xbc{g}", [128, 515]) for g in range(3)]
        xbcB = [Buf() for g in range(3)]
        for h in range(2):
            G("memset", dict(ap=pre[h][:], constant=0.0), w=[padB[h]])
            G("memset", dict(ap=pad[h][:], constant=0.0), w=[padB[h]])
            G("memset", dict(ap=hTf[h][:], constant=0.0), w=[hfB[h]])
            G("memset", dict(ap=hTb[h][:], constant=0.0), w=[hbB[h]])
        G("memset", dict(ap=Vaug[:, :, :, 64:65], constant=1.0), w=vB_)
        G("memset", dict(ap=carry[:], constant=0.0), w=ccB)
        for g in range(3):
            G("memset", dict(ap=xbc[g][:, 0:3], constant=0.0), w=[xbcB[g]])

        xts = [sb(f"xt{k}", [128, NCH, 256]) for k in range(2)]
        xtBs = [Buf(), Buf()]
        sq = [sb(f"sq{k}", [128, 512], BF16) for k in range(2)]
        sqB = [Buf(), Buf()]
        rstd = sb("rstd", [128, 512])
        lnv = rstd
        rsB = Buf()
        hT = sb("hT", [128, NCH, 512], BF16)
        hB = Buf()
        cacc1 = sb("cacc", [128, 512])
        cacc = [cacc1] * 3
        caB = [Buf()] * 3
        zs = [sb(f"zs{p}", [128, 512]) for p in range(2)]
        zsB = [Buf(), Buf()]
        xcv = [[sb(f"xcv{p}{g}", [128, 512], BF16) for g in range(3)] for p in range(2)]
        xcB = [[Buf() for g in range(3)] for p in range(2)]
        q2 = [[sb(f"q2{p}{h}", [128, 512], BF16) for h in range(2)] for p in range(2)]
        q2B = [Buf(), Buf()]
        drow = [[sb(f"drow{p}{h}", [128, 512], BF16) for h in range(2)] for p in range(2)]
        drB = [[Buf(), Buf()] for p in range(2)]
        for p_ in range(2):
            for h_ in range(2):
                G("memset", dict(ap=q2[p_][h_][:], constant=0.0), w=[q2B[p_]])
                G("memset", dict(ap=drow[p_][h_][:], constant=0.0), w=[drB[p_][h_]])
        dtc = [sb(f"dtc{p}", [128, 4, 2]) for p in range(2)]
        acol = [sb(f"acol{p}", [128, 4, 2]) for p in range(2)]
        colB = [Buf(), Buf()]
        biasT = [[sb(f"biasT{p}{h}", [128, NB]) for h in range(2)] for p in range(2)]
        biB = [[Buf(), Buf()] for p in range(2)]
        sm = sb("sm", [128, 4, 4])
        smB = Buf()
        e1 = sb("e1", [128, 4, 4])
        LF = sb("LF", [128, 2, 4])
        lfB = Buf()
        ptile = [sb(f"ptile{k}", [128, 512], BF16) for k in range(3)]
        ptB = [Buf() for k in range(3)]
        oT = sb("oT", [65, 512])
        oTB = Buf()
        rec = sb("rec", [64, 512])
        recB = Buf()
        yfx = rec
        yfxB = recB
        ygt = sb("ygt", [128, 512])
        ygB = Buf()
        xs_tok = [sb(f"xstok{k}", [128, 128]) for k in range(2)]
        xdt = [sb(f"xdt{k}", [128, 128], BF16) for k in range(2)]
        Btok = [sb(f"Btok{k}", [128, 128], BF16) for k in range(2)]
        tkB = [Buf(), Buf()]
        cbm = [sb(f"cbm{k}", [128, 128]) for k in range(2)]
        cbmB = [Buf(), Buf()]
        Wa = [[sb(f"Wa{k}{h}", [128, 256]) for h in range(2)] for k in range(2)]
        WaB = [[Buf(), Buf()] for k in range(2)]
        E = [[sb(f"E{k}{h}", [128, 256]) for h in range(2)] for k in range(2)]
        EB = [[Buf(), Buf()] for k in range(2)]
        MT = [[sb(f"MT{k}{h}", [128, 128], BF16) for h in range(2)] for k in range(2)]
        Cp = [[sb(f"Cp{k}{h}", [128, 128], BF16) for h in range(2)] for k in range(2)]
        xdtd = [[sb(f"xdtd{k}{h}", [128, 64], BF16) for h in range(2)] for k in range(2)]
        mB = [[Buf(), Buf()] for k in range(2)]
        ytok = [sb(f"ytok{k}", [128, 128]) for k in range(2)]
        ytB = [Buf(), Buf()]

        bigL = [pt(f"bigL{k}", [128, 512]) for k in range(2)]
        bigLB = [Buf() for k in range(2)]
        bigF = [pt(f"bigF{k}", [128, 512]) for k in range(2)]
        bigFB = [Buf() for k in range(2)]
        cnt = {"L": 0, "F": 0, "pt": 0}

        def nextL():
            k = cnt["L"] % 2
            cnt["L"] += 1
            return bigL[k], bigLB[k]

        def nextF():
            k = cnt["F"] % 2
            cnt["F"] += 1
            return bigF[k], bigFB[k]
        po = pt("po", [128, 512])
        poB = Buf()
        tp = pt("tp", [128, 256], BF16)
        tpB = Buf()
        ssd1 = pt("ssd1", [128, 512])
        pcb = ssd1[:, 0:128]
        pcbB = Buf()
        py = ssd1[:, 128:256]
        pyB = pcbB
        pst = ssd1[:, 256:384]
        pstB = pcbB
        pyt = ssd1[:, 384:512]
        pytB = pcbB
        psegt = pt("psegt", [128, 512])
        pseg = [psegt[:, 256 * h:256 * h + 256] for h in range(2)]
        psegB = [Buf()] * 2

        def gen_L(i):
            p = i % 2
            cols = slice(i * 512, (i + 1) * 512)
            for half in range(2):
                h0 = half * 256
                xt, xtB = xts[half], xtBs[half]
                for q in range(4):
                    P.dma("sync", xt[:, 4 * q:4 * q + 4, :],
                          xTv[:, 4 * q:4 * q + 4, i * 512 + h0:i * 512 + h0 + 256], writes=[xtB])
                pss, pssB = nextL()
                for c in range(NCH):
                    k = c % 2
                    G("tensor_tensor", dict(out=sq[k][:, 0:256], in0=xt[:, c, :], in1=xt[:, c, :], op=ALU.mult),
                      r=[xtB], w=[sqB[k]])
                    P.mm(pss[:, 0:256], onesb[:], sq[k][:, 0:256], c == 0, c == NCH - 1,
                         reads=[sqB[k], cB], writes=[pssB])
                yield
                A("activation", dict(out=lnv[:, 0:256], in_=pss[:, 0:256], func=AF.Ln, scale=1.0 / D, bias=1e-6),
                  r=[pssB], w=[rsB])
                A("activation", dict(out=rstd[:, 0:256], in_=lnv[:, 0:256], func=AF.Exp, scale=-0.5),
                  r=[rsB], w=[rsB])
                for c in range(NCH):
                    V("tensor_tensor", dict(out=hT[:, c, h0:h0 + 256], in0=xt[:, c, :], in1=rstd[:, 0:256],
                                            op=ALU.mult), r=[xtB, rsB], w=[hB])
                yield
            def fm_post(name, ps, psB):
                if name == "z":
                    A("activation", dict(out=zs[p][:], in_=ps[:], func=AF.Silu), r=[psB], w=[zsB[p]])
                elif name in ("xs", "B", "C"):
                    g = ("xs", "B", "C").index(name)
                    A("copy", dict(out=xbc[g][:, 3:515], in_=ps[:]), r=[psB], w=[xbcB[g]])
                elif name == "q":
                    A("mul", dict(out=q2[p][0][0:64, :], in_=ps[0:64, :], mul=0.125), r=[psB], w=[q2B[p]])
                    A("mul", dict(out=q2[p][1][64:128, :], in_=ps[64:128, :], mul=0.125), r=[psB], w=[q2B[p]])
                else:
                    V("tensor_copy", dict(out=k2[:, cols], in_=ps[:]), r=[psB], w=[kB_[i]])

            def fm_conv(name):
                if name in ("xs", "B", "C"):
                    g = ("xs", "B", "C").index(name)
                    V("tensor_scalar", dict(out=cacc[g][:], in0=xbc[g][:, 0:512],
                                            scalar1=cw[:, 4 * g:4 * g + 1], scalar2=None, op0=ALU.mult),
                      r=[xbcB[g], cB], w=[caB[g]])
                    for kk in range(1, 4):
                        V("scalar_tensor_tensor", dict(
                            out=cacc[g][:], in0=xbc[g][:, kk:kk + 512], scalar=cw[:, 4 * g + kk:4 * g + kk + 1],
                            in1=cacc[g][:], op0=ALU.mult, op1=ALU.add), r=[xbcB[g], cB], w=[caB[g]])
                    V("tensor_copy", dict(out=xbc[g][:, 0:3], in_=xbc[g][:, 512:515]),
                      r=[xbcB[g]], w=[xbcB[g]])

            def fm_silu(name):
                if name in ("xs", "B", "C"):
                    g = ("xs", "B", "C").index(name)
                    A("activation", dict(out=xcv[p][g][:], in_=cacc[g][:], func=AF.Silu,
                                         bias=cb[:, g:g + 1]), r=[caB[g], cB], w=[xcB[p][g]])

            pipe = []
            for (name, c0, wd) in FM + [(None, 0, 0)] * 3:
                nxt = []
                for (stg_, nm, ps_, psB_) in pipe:
                    if stg_ == 1:
                        fm_post(nm, ps_, psB_)
                        nxt.append((2, nm, None, None))
                    elif stg_ == 2:
                        fm_conv(nm)
                        nxt.append((3, nm, None, None))
                    else:
                        fm_silu(nm)
                pipe = nxt
                if name is not None:
                    ps, psB = nextL()
                    for c in range(NCH):
                        P.mm(ps[0:wd, :], W[:, c, c0:c0 + wd], hT[:, c, :], c == 0, c == NCH - 1,
                             reads=[wB, hB], writes=[psB])
                    pipe.append((1, name, ps, psB))
                yield
            pend_tm = None
            for b in range(5):
                if b < 4:
                    blk = 4 * i + b
                    ps, psB = nextL()
                    for c in range(NCH):
                        P.mm(ps[:, 0:132], hT[:, c, b * 128:(b + 1) * 128], W[:, c, TM0:TM0 + 132],
                             c == 0, c == NCH - 1, reads=[wB, hB], writes=[psB])
                if pend_tm is not None:
                    (pb, pblk, pps, ppsB) = pend_tm
                    V("tensor_copy", dict(out=Vaug[:, pblk, :, 0:64],
                                          in_=pps[:, 0:128].rearrange("p (h d) -> p h d", h=2)),
                      r=[ppsB], w=[vB_[i]])
                    V("tensor_copy", dict(out=sm[:, pb, :], in_=pps[:, 128:132]), r=[ppsB], w=[smB])
                pend_tm = (b, blk, ps, psB) if b < 4 else None
                yield
            for h in range(2):
                A("activation", dict(out=e1[:, :, h], in_=sm[:, :, h], func=AF.Exp,
                                     bias=hp[:, h:h + 1]), r=[smB, cB], w=[lfB])
                A("activation", dict(out=e1[:, :, 2 + h], in_=sm[:, :, 2 + h], func=AF.Exp,
                                     scale=-1.0, bias=hq[:, 2 + h:3 + h]), r=[smB, cB], w=[lfB])
            yield
            A("activation", dict(out=e1[:], in_=e1[:], func=AF.Ln, bias=1.0), r=[lfB], w=[lfB])
            yield
            V("tensor_copy", dict(out=dtc[p][:], in_=e1[:, :, 0:2]), r=[lfB], w=[colB[p]])
            for h in range(2):
                V("tensor_scalar", dict(out=acol[p][:, :, h], in0=e1[:, :, h], scalar1=hq[:, h:h + 1],
                                        scalar2=None, op0=ALU.mult), r=[lfB, cB], w=[colB[p]])
                V("tensor_scalar", dict(out=LF[:, h, :], in0=e1[:, :, 2 + h], scalar1=-1.0,
                                        scalar2=None, op0=ALU.mult), r=[lfB], w=[lfB])
            nkb = 4 * i + 4
            for h in range(2):
                pcf, pcB_ = nextL()
                pc = pcf[:, 0:5]
                for b in range(4):
                    V("tensor_tensor", dict(out=pre[h][:, b + 1:b + 2], in0=pre[h][:, b:b + 1],
                                            in1=LF[:, h, b:b + 1], op=ALU.add),
                      r=[lfB, padB[h]], w=[padB[h]])
                yield
                P.mm(pc[:, 0:4], U[:], LF[:, h, :], True, False, reads=[lfB, cB], writes=[pcB_])
                P.mm(pc[:, 0:4], ones[:], pre[h][:, 0:4], False, True, reads=[padB[h], cB], writes=[pcB_])
                P.mm(pc[:, 4:5], ones[:], pre[h][:, 4:5], True, True, reads=[padB[h], cB], writes=[pcB_])
                yield
                V("tensor_scalar", dict(out=Ccol[:, h, 4 * i:4 * i + 4], in0=pc[:, 0:4],
                                        scalar1=carry[:, h:h + 1], scalar2=None, op0=ALU.add),
                  r=[pcB_], w=[ccB[h]])
                V("tensor_scalar", dict(out=biasT[p][h][:, 0:nkb], in0=Ccol[:, h, 0:nkb],
                                        scalar1=carry[:, h:h + 1], scalar2=-1.0,
                                        op0=ALU.subtract, op1=ALU.mult),
                  r=[ccB[h]], w=[biB[p][h]])
                V("tensor_copy", dict(out=pad[h][:], in_=pc[:, 0:4]),
                  r=[pcB_], w=[padB[h]])
                V("tensor_tensor", dict(out=carry[:, h:h + 1], in0=carry[:, h:h + 1],
                                        in1=pc[:, 4:5], op=ALU.add),
                  r=[pcB_, biB[p][h]], w=[ccB[h]])
                yield
                pa, paB = nextL()
                for b in range(4):
                    P.mm(pa[0:1, b * 128:(b + 1) * 128], pad[h][:, b:b + 1], idb[:], True, True,
                         reads=[padB[h], cB], writes=[paB])
                yield
                A("copy", dict(out=drow[p][h][0:1, :], in_=pa[0:1, :]), r=[paB], w=[drB[p][h]])
                yield

        def gen_S(i):
            p = i % 2
            cols = slice(i * 512, (i + 1) * 512)
            for b in range(4):
                k = b % 2
                bc = slice(b * 128, (b + 1) * 128)
                P.opk("tensor", "transpose", dict(out=tp[:, 0:128], in_=xcv[p][0][:, bc], identity=idb[:]),
                      [xcB[p][0], cB], [tpB])
                P.opk("tensor", "transpose", dict(out=tp[:, 128:256], in_=xcv[p][1][:, bc], identity=idb[:]),
                      [xcB[p][1], cB], [tpB])
                P.mm(pcb, xcv[p][1][:, bc], xcv[p][2][:, bc], True, True, reads=[xcB[p][1], xcB[p][2]],
                     writes=[pcbB])
                for h in range(2):
                    G("tensor_scalar", dict(
                        out=Wa[k][h][:, 0:128], in0=SL[:], scalar1=acol[p][:, b, h:h + 1], scalar2=None,
                        op0=ALU.mult), r=[colB[p], cB], w=[WaB[k][h]])
                    G("tensor_scalar", dict(
                        out=Wa[k][h][:, 128:256], in0=ones[:], scalar1=acol[p][:, b, h:h + 1], scalar2=None,
                        op0=ALU.mult), r=[colB[p], cB], w=[WaB[k][h]])
                yield
                A("copy", dict(out=xs_tok[k][:], in_=tp[:, 0:128]), r=[tpB], w=[tkB[k]])
                for h in range(2):
                    hc = slice(h * 64, (h + 1) * 64)
                    V("tensor_scalar", dict(
                        out=xdt[k][:, hc], in0=tp[:, hc], scalar1=dtc[p][:, b, h:h + 1], scalar2=None,
                        op0=ALU.mult), r=[tpB, colB[p]], w=[tkB[k]])
                A("copy", dict(out=Btok[k][:], in_=tp[:, 128:256]), r=[tpB], w=[tkB[k]])
                V("tensor_tensor", dict(out=cbm[k][:], in0=pcb, in1=U[:], op=ALU.mult),
                  r=[pcbB, cB], w=[cbmB[k]])
                for h in range(2):
                    P.mm(pseg[h][:, 0:128], Wa[k][h][:, 0:128], U[:], True, True, reads=[WaB[k][h], cB],
                         writes=[psegB[h]])
                    P.mm(pseg[h][:, 128:256], Wa[k][h][:, 128:256], U[:], True, True, reads=[WaB[k][h], cB],
                         writes=[psegB[h]])
                yield
                for h in range(2):
                    A("activation", dict(out=E[k][h][:], in_=pseg[h][:], func=AF.Exp),
                      r=[psegB[h]], w=[EB[k][h]])
                yield
                for h in range(2):
                    hc = slice(h * 64, (h + 1) * 64)
                    V("tensor_tensor", dict(out=MT[k][h][:], in0=E[k][h][:, 0:128], in1=cbm[k][:],
                                            op=ALU.mult), r=[EB[k][h], cbmB[k]], w=[mB[k][h]])
                    G("tensor_tensor", dict(out=Cp[k][h][:], in0=xcv[p][2][:, bc],
                                            in1=E[k][h][:, 128:256], op=ALU.mult),
                      r=[EB[k][h], xcB[p][2]], w=[mB[k][h]])
                    G("tensor_scalar", dict(out=xdtd[k][h][:], in0=xdt[k][:, hc],
                                            scalar1=E[k][h][:, 127:128], scalar2=None,
                                            op0=ALU.mult),
                      r=[EB[k][h], tkB[k]], w=[mB[k][h]])
                yield
                for h in range(2):
                    hc = slice(h * 64, (h + 1) * 64)
                    P.mm(py[:, hc], MT[k][h][:], xdt[k][:, hc], True, False, reads=[mB[k][h], tkB[k]],
                         writes=[pyB])
                    P.mm(py[:, hc], Cp[k][h][:], hTb[h][:], False, True, reads=[mB[k][h], hbB[h]], writes=[pyB])
                    P.mm(pst[:, hc], Btok[k][:], xdtd[k][h][:], True, True, reads=[tkB[k], mB[k][h]],
                         writes=[pstB])
                yield
                for h in range(2):
                    hc = slice(h * 64, (h + 1) * 64)
                    V("scalar_tensor_tensor", dict(
                        out=hTf[h][:], in0=hTf[h][:], scalar=E[k][h][:, 255:256], in1=pst[:, hc],
                        op0=ALU.mult, op1=ALU.add), r=[pstB, EB[k][h]], w=[hfB[h]])
                    V("tensor_copy", dict(out=hTb[h][:], in_=hTf[h][:]), r=[hfB[h]], w=[hbB[h]])
                    V("scalar_tensor_tensor", dict(
                        out=ytok[k][:, hc], in0=xs_tok[k][:, hc], scalar=hp[:, 4 + h:5 + h], in1=py[:, hc],
                        op0=ALU.mult, op1=ALU.add), r=[pyB, tkB[k], cB], w=[ytB[k]])
                yield
                P.opk("tensor", "transpose", dict(out=pyt, in_=ytok[k][:], identity=idf[:]),
                      [ytB[k], cB], [pytB])
                yield
                V("tensor_tensor", dict(out=ygt[:, bc], in0=pyt, in1=zs[p][:, bc],
                                        op=ALU.mult), r=[pytB, zsB[p]], w=[ygB])
                yield
            P.dma("sync", yT[0:128, cols], ygt[:], reads=[ygB])
            yield

        def gen_F(i):
            p = i % 2
            cols = slice(i * 512, (i + 1) * 512)
            nkb = 4 * i + 4
            for h in range(2):
                last = nkb - 1
                hp0 = 64 * h
                pend = []
                LAG = 2
                for kb in range(nkb + LAG):
                    if kb < nkb:
                        j = kb - 4 * i
                        q0 = 0 if j < 0 else j * 128
                        N = 512 - q0
                        ps, psB = nextF()
                        kk = cnt["pt"] % 3
                        cnt["pt"] += 1
                        P.mm(ps[:, 0:N], k2[:, kb * 128:(kb + 1) * 128], q2[p][h][:, q0:512],
                             True, False, reads=[kB_[kb // 4], q2B[p]], writes=[psB])
                        P.mm(ps[:, 0:N], onesb[:], drow[p][h][:, q0:512], False, True,
                             reads=[cB, drB[p][h]], writes=[psB])
                        A("activation", dict(
                            out=ptile[kk][:, 0:N], in_=ps[:, 0:N], func=AF.Exp, bias=biasT[p][h][:, kb:kb + 1]),
                          r=[psB, biB[p][h]], w=[ptB[kk]])
                        if j >= 0:
                            G("affine_select", dict(
                                out=ptile[kk][:, 0:128], in_=ptile[kk][:, 0:128], pattern=[[1, 128]],
                                compare_op=ALU.is_ge, fill=0.0, base=0, channel_multiplier=-1),
                              r=[ptB[kk]], w=[ptB[kk]])
                        pend.append((kb, q0, N, kk))
                    if kb >= LAG or kb >= nkb:
                        if pend and (kb >= nkb or len(pend) > LAG):
                            (pkb, pq0, pN, pkk) = pend.pop(0)
                            P.mm(po[0:65, pq0:512], Vaug[:, pkb, h, 0:65], ptile[pkk][:, 0:pN], pkb == 0, pkb == last,
                                 reads=[vB_[pkb // 4], ptB[pkk]], writes=[poB])
                    yield
                while pend:
                    (pkb, pq0, pN, pkk) = pend.pop(0)
                    P.mm(po[0:65, pq0:512], Vaug[:, pkb, h, 0:65], ptile[pkk][:, 0:pN], pkb == 0, pkb == last,
                         reads=[vB_[pkb // 4], ptB[pkk]], writes=[poB])
                yield
                A("copy", dict(out=oT[:], in_=po[0:65, :]), r=[poB], w=[oTB])
                yield
                P.mm(po[0:64, :], ones[64:65, 0:64], oT[64:65, :], True, True, reads=[oTB, cB], writes=[poB])
                yield
                V("reciprocal", dict(out=rec[:], in_=po[0:64, :]), r=[poB], w=[recB])
                V("tensor_tensor", dict(out=yfx[:], in0=oT[0:64, :], in1=rec[:], op=ALU.mult),
                  r=[oTB, recB], w=[yfxB])
                P.dma("sync", yT[128 + 64 * h:192 + 64 * h, cols], yfx[:], reads=[yfxB])
                yield

        def run_all(g):
            for _ in g:
                pass

        def merge(gens, weights):
            live = list(gens)
            wts = list(weights)
            while live:
                for idx in range(len(live) - 1, -1, -1):
                    pass
                nxt_live, nxt_w = [], []
                for g, wgt in zip(live, wts):
                    done = False
                    for _ in range(wgt):
                        try:
                            next(g)
                        except StopIteration:
                            done = True
                            break
                    if not done:
                        nxt_live.append(g)
                        nxt_w.append(wgt)
                live, wts = nxt_live, nxt_w

        run_all(gen_L(0))
        for i in range(NT):
            import os
            skip = os.environ.get("P1SKIP", "")
            gens, wts = [], []
            if "F" not in skip:
                gens.append(gen_F(i)); wts.append(max(1, (8 * i + 16) // 34))
            if "S" not in skip:
                gens.append(gen_S(i)); wts.append(1)
            if i + 1 < NT:
                gens.append(gen_L(i + 1))
                wts.append(1)
            merge(gens, wts)
        P.finish()
        P.emit()
    return nc


NEG = -1.0e30


def build_p2(TT, dbg=False, stages=(1, 2, 3, 4)):
    NTL = TT // 512
    NBL = TT // 128
    nc = bass.Bass("TRN2", target_bir_lowering=False)
    dram = lambda n, s, dt=F32, kind="ExternalInput": nc.dram_tensor(n, s, dt, kind=kind).ap()
    yTd = dram("yTin", [D, TT])
    xTd = dram("xTin", [D, TT])
    wod = dram("wo", [D, D])
    wqd = dram("wq", [D, D])
    snwd = dram("snw", [128, 8])
    ln2d = dram("ln2", [128, NCH])
    lnfd = dram("lnf", [128, NCH])
    k1Td = dram("k1T", [128, 8, 128])
    k2Td = dram("k2T", [128, 8, 128])
    uTd = dram("uT", [128, 128, D])
    vd = dram("vv", [16384, D])
    outT = dram("outT", [D, TT], kind="ExternalOutput")
    x1s = dram("x1s", [D, TT], kind="ExternalOutput" if dbg else "Internal")
    h2s = dram("h2s", [D, TT], BF16, kind="Internal")
    Sd = dram("Sd", [TT, 2048], kind="Internal")
    Gd = dram("Gd", [128, 128, TT], BF16, kind="ExternalOutput" if dbg else "Internal")
    v3 = lambda a: a.rearrange("(c p) t -> p c t", p=128)
    yTv, xTv, x1v, h2v, outv = v3(yTd), v3(xTd), v3(x1s), v3(h2s), v3(outT)
    wov = wod.rearrange("(c p) n -> p c n", p=128)
    wqv = wqd.rearrange("(c p) n -> p c n", p=128)
    ubd = dram("ubd", [128, 128, D], BF16, kind="Internal")
    vbd = dram("vbd", [128, 128, D], BF16, kind="Internal")

    with ExitStack() as es0:
        P = Prog(nc, es0)
        V = lambda f, kw, r=(), w=(): P.opk("vector", f, kw, r, w)
        G = lambda f, kw, r=(), w=(): P.opk("gpsimd", f, kw, r, w)
        A = lambda f, kw, r=(), w=(): P.opk("scalar", f, kw, r, w)

        stg = [0]

        def mk(es):
            stg[0] += 1
            pre = f"s{stg[0]}_"
            sb = lambda name, shape, dt=F32: es.enter_context(nc.sbuf_tensor(pre + name, shape, dt))
            pt = lambda name, shape, dt=F32: es.enter_context(nc.psum_tensor(pre + name, shape, dt))
            return sb, pt

        sb0, _ = mk(es0)
        cB = Buf("const")
        onesb = sb0("onesb", [128, 128], BF16)
        idb = sb0("idb", [128, 128], BF16)
        idf = sb0("idf", [128, 128])
        ln2t = sb0("ln2t", [128, NCH])
        lnft = sb0("lnft", [128, NCH])
        snwt = sb0("snwt", [128, 8])
        G("memset", dict(ap=onesb[:], constant=1.0), w=[cB])
        G("memset", dict(ap=idf[:], constant=1.0), w=[cB])
        G("affine_select", dict(out=idf[:], in_=idf[:], pattern=[[1, 128]], compare_op=ALU.is_equal,
                                fill=0.0, base=0, channel_multiplier=-1), w=[cB])
        G("tensor_copy", dict(out=idb[:], in_=idf[:]), r=[cB], w=[cB])
        P.dma("sync", ln2t[:], ln2d, writes=[cB])
        P.dma("sync", lnft[:], lnfd, writes=[cB])
        P.dma("sync", snwt[:], snwd, writes=[cB])

        def load_weight(sb, Wt, wB, wv, scale_t, nscaled, name):
            wst = [sb(f"{name}st{k}", [128, D]) for k in range(2)]
            wstB = [Buf(), Buf()]
            for c in range(NCH):
                k = c % 2
                P.dma("sync", wst[k][:], wv[:, c, :], writes=[wstB[k]])
                if c < nscaled:
                    V("tensor_scalar", dict(out=Wt[:, c, :], in0=wst[k][:], scalar1=scale_t[:, c:c + 1],
                                            scalar2=None, op0=ALU.mult), r=[wstB[k], cB], w=[wB])
                else:
                    A("copy", dict(out=Wt[:, c, :], in_=wst[k][:]), r=[wstB[k]], w=[wB])

        def rms_bcast(pss, pssB, src_chunks, srcB, sq, sqB, n, width):
            for ci, src in enumerate(src_chunks):
                k = ci % 2
                A("activation", dict(out=sq[k][:, 0:width], in_=src, func=AF.Square), r=[srcB], w=[sqB[k]])
                P.mm(pss[:, 0:width], onesb[:], sq[k][:, 0:width], ci == 0, ci == n - 1,
                     reads=[sqB[k], cB], writes=[pssB])

        if 1 in stages:
            with ExitStack() as es:
                sb, pt = mk(es)
                Wo = sb("Wo", [128, NCH, D], BF16)
                woB = Buf()
                load_weight(sb, Wo, woB, wov, snwt, 8, "wo")
                yt = sb("yt", [128, 4, 512])
                ytB = Buf()
                ynT = sb("ynT", [128, NCH, 512], BF16)
                ynB = Buf()
                sq = [sb(f"sq{k}", [128, 512], BF16) for k in range(2)]
                sqB = [Buf(), Buf()]
                rs = sb("rs", [128, 512])
                rsB = Buf()
                xc = [sb(f"xc{k}", [128, 512]) for k in range(2)]
                xcB = [Buf(), Buf()]
                x1T = sb("x1T", [128, NCH, 512])
                x1B = Buf()
                h2t = sb("h2t", [128, NCH, 512], BF16)
                h2B = Buf()
                acc = [pt(f"acc{k}", [128, 512]) for k in range(4)]
                accB = [Buf() for k in range(4)]
                an = [0]

                def nacc():
                    k = an[0] % 4
                    an[0] += 1
                    return acc[k], accB[k]
                for j in range(NTL):
                    cols = slice(j * 512, (j + 1) * 512)
                    for g4 in range(4):
                        P.dma("sync", yt[:], yTv[:, 4 * g4:4 * g4 + 4, cols], writes=[ytB])
                        if g4 < 2:
                            pss, pssB = nacc()
                            rms_bcast(pss, pssB, [yt[:, c, :] for c in range(4)], ytB, sq, sqB, 4, 512)
                            A("activation", dict(out=rs[:], in_=pss[:], func=AF.Ln, scale=1.0 / 512, bias=1e-6),
                              r=[pssB], w=[rsB])
                            A("activation", dict(out=rs[:], in_=rs[:], func=AF.Exp, scale=-0.5), r=[rsB], w=[rsB])
                            for c in range(4):
                                V("tensor_tensor", dict(out=ynT[:, 4 * g4 + c, :], in0=yt[:, c, :], in1=rs[:],
                                                        op=ALU.mult), r=[ytB, rsB], w=[ynB])
                        else:
                            for c in range(4):
                                V("tensor_copy", dict(out=ynT[:, 4 * g4 + c, :], in_=yt[:, c, :]), r=[ytB], w=[ynB])
                    for dc in range(NCH):
                        k = dc % 2
                        P.dma("sync", xc[k][:], xTv[:, dc, cols], writes=[xcB[k]])
                        ps, psB = nacc()
                        for c in range(NCH):
                            P.mm(ps[:], Wo[:, c, dc * 128:(dc + 1) * 128], ynT[:, c, :], c == 0, c == NCH - 1,
                                 reads=[woB, ynB], writes=[psB])
                        V("tensor_tensor", dict(out=x1T[:, dc, :], in0=ps[:], in1=xc[k][:], op=ALU.add),
                          r=[psB, xcB[k]], w=[x1B])
                    P.dma("sync", x1v[:, :, cols], x1T[:], reads=[x1B])
                    pss, pssB = nacc()
                    rms_bcast(pss, pssB, [x1T[:, c, :] for c in range(NCH)], x1B, sq, sqB, NCH, 512)
                    A("activation", dict(out=rs[:], in_=pss[:], func=AF.Ln, scale=1.0 / D, bias=1e-6),
                      r=[pssB], w=[rsB])
                    A("activation", dict(out=rs[:], in_=rs[:], func=AF.Exp, scale=-0.5), r=[rsB], w=[rsB])
                    for c in range(NCH):
                        V("tensor_tensor", dict(out=h2t[:, c, :], in0=x1T[:, c, :], in1=rs[:], op=ALU.mult),
                          r=[x1B, rsB], w=[h2B])
                    P.dma("sync", h2v[:, :, cols], h2t[:], reads=[h2B])
                P.barrier()
                P.emit()

        if 2 in stages:
            with ExitStack() as es:
                sb, pt = mk(es)
                Wq = sb("Wq", [128, NCH, D], BF16)
                wqB = Buf()
                load_weight(sb, Wq, wqB, wqv, ln2t, NCH, "wq")
                kst = sb("kst", [128, 8, 128])
                kT = [sb(f"kT{i}", [128, 8, 128], BF16) for i in range(2)]
                kB = Buf()
                for i, kd in enumerate((k1Td, k2Td)):
                    P.dma("sync", kst[:], kd, writes=[kB])
                    V("tensor_copy", dict(out=kT[i][:], in_=kst[:]), r=[kB], w=[kB])
                h2t = sb("h2t", [128, NCH, 512], BF16)
                h2B = Buf()
                qpT = sb("qpT", [128, NCH, 512], BF16)
                qpB = Buf()
                sall = [sb(f"sall{k}", [128, 2048]) for k in range(2)]
                saB = [Buf(), Buf()]
                acc = [pt(f"acc{k}", [128, 512]) for k in range(4)]
                accB = [Buf() for k in range(4)]
                an = [0]

                def nacc():
                    k = an[0] % 4
                    an[0] += 1
                    return acc[k], accB[k]
                for j in range(NTL):
                    cols = slice(j * 512, (j + 1) * 512)
                    P.dma("sync", h2t[:], h2v[:, :, cols], writes=[h2B])
                    for cc in range(NCH):
                        ps, psB = nacc()
                        for c in range(NCH):
                            P.mm(ps[:], Wq[:, c, cc * 128:(cc + 1) * 128], h2t[:, c, :], c == 0, c == NCH - 1,
                                 reads=[wqB, h2B], writes=[psB])
                        if cc % 2 == 0:
                            A("copy", dict(out=qpT[:, cc, :], in_=ps[:]), r=[psB], w=[qpB])
                        else:
                            V("tensor_copy", dict(out=qpT[:, cc, :], in_=ps[:]), r=[psB], w=[qpB])
                    for b in range(4):
                        k = b % 2
                        bc = slice(b * 128, (b + 1) * 128)
                        for q4 in range(4):
                            ps, psB = nacc()
                            for c4 in range(4):
                                cc = 4 * q4 + c4
                                P.mm(ps[:, c4 * 128:(c4 + 1) * 128], qpT[:, cc, bc], kT[cc % 2][:, cc // 2, :],
                                     True, True, reads=[qpB, kB], writes=[psB])
                            if q4 % 2 == 0:
                                A("copy", dict(out=sall[k][:, q4 * 512:(q4 + 1) * 512], in_=ps[:]), r=[psB], w=[saB[k]])
                            else:
                                V("tensor_copy", dict(out=sall[k][:, q4 * 512:(q4 + 1) * 512], in_=ps[:]),
                                  r=[psB], w=[saB[k]])
                        t0 = j * 512 + b * 128
                        P.dma("sync", Sd[t0:t0 + 128, :], sall[k][:], reads=[saB[k]])
                P.barrier()
                P.emit()

        if 3 in stages:
            with ExitStack() as es:
                sb, pt = mk(es)
                sall = [sb(f"sall{p}", [128, 8, 2, 128]) for p in range(2)]
                saB = [Buf(), Buf()]
                top = [sb(f"top{p}", [128, 8, 2, 16]) for p in range(2)]
                topB = [Buf(), Buf()]
                sm8 = [sb(f"sm8{p}", [128, 8 * 8]) for p in range(2)]
                rB = [Buf(), Buf()]
                swork = sb("swork", [128, 128])
                cand = sb("cand", [128, 8, 256])
                cwork = sb("cwork", [128, 256])
                c16 = sb("c16", [128, 8, 16])
                cdB = Buf()
                smm = [sb(f"smm{k}", [128, 16, 128]) for k in range(2)]
                smB = [Buf(), Buf()]
                msk = [sb(f"msk{k}", [128, 16, 128], BF16) for k in range(2)]
                mkB = [Buf(), Buf()]
                ex = [sb(f"ex{k}", [128, 16, 128]) for k in range(2)]
                exB = [Buf(), Buf()]
                R = sb("R", [128, 128, 128], BF16)
                RB = Buf()
                OH = sb("OH", [128, 128, 128], BF16)
                OHB = Buf()
                RT = sb("RT", [128, 128, 64], BF16)
                RTB = Buf()
                PT = sb("PT", [128, 128, 64], BF16)
                PTB = Buf()
                Gs2 = [sb(f"Gs{k}", [128, 128, 64], BF16) for k in range(2)]
                Gs2B = [Buf(), Buf()]
                tpp = [pt(f"tpp{k}", [128, 1024], BF16) for k in range(3)]
                tpB = [Buf() for k in range(3)]
                pg = [pt(f"pg{k}", [128, 512]) for k in range(3)]
                pgB = [Buf() for k in range(3)]
                rr = {"tp": 0, "pg": 0}

                def gen_X(blk):
                    p = blk % 2
                    t0 = blk * 128
                    thr, mx, negm, Z, lnZ, adj = [sm8[p][:, 8 * i:8 * i + 8] for i in range(6)]
                    P.dma("sync", sall[p][:], Sd[t0:t0 + 128, :].rearrange("t (h f n) -> t h f n", h=8, f=2),
                          writes=[saB[p]])
                    for h in range(8):
                        for f in range(2):
                            V("max", dict(out=top[p][:, h, f, 0:8], in_=sall[p][:, h, f, :]), r=[saB[p]], w=[topB[p]])
                            V("match_replace", dict(out=swork[:], in_to_replace=top[p][:, h, f, 0:8],
                                                    in_values=sall[p][:, h, f, :], imm_value=NEG),
                              r=[saB[p], topB[p]], w=[cdB])
                            V("max", dict(out=top[p][:, h, f, 8:16], in_=swork[:]), r=[cdB], w=[topB[p]])
                        yield
                    V("tensor_tensor", dict(out=cand[:].rearrange("p h (a b) -> p h a b", a=16),
                                            in0=top[p][:, :, 0, :].unsqueeze(3).to_broadcast([128, 8, 16, 16]),
                                            in1=top[p][:, :, 1, :].unsqueeze(2).to_broadcast([128, 8, 16, 16]),
                                            op=ALU.add), r=[topB[p]], w=[cdB])
                    for h in range(8):
                        V("max", dict(out=c16[:, h, 0:8], in_=cand[:, h, :]), r=[cdB], w=[cdB])
                        V("match_replace", dict(out=cwork[:], in_to_replace=c16[:, h, 0:8], in_values=cand[:, h, :],
                                                imm_value=NEG), r=[cdB], w=[cdB])
                        V("max", dict(out=c16[:, h, 8:16], in_=cwork[:]), r=[cdB], w=[cdB])
                        if h % 2 == 1:
                            yield
                    V("tensor_reduce", dict(out=thr, in_=c16[:], axis=AX.X, op=ALU.min), r=[cdB], w=[rB[p]])
                    V("tensor_reduce", dict(out=mx, in_=c16[:], axis=AX.X, op=ALU.max), r=[cdB], w=[rB[p]])
                    V("tensor_scalar", dict(out=negm, in0=mx, scalar1=-1.0, scalar2=None, op0=ALU.mult),
                      r=[rB[p]], w=[rB[p]])
                    V("tensor_tensor", dict(out=c16[:], in0=c16[:], in1=mx.unsqueeze(2).to_broadcast([128, 8, 16]),
                                            op=ALU.subtract), r=[cdB, rB[p]], w=[cdB])
                    A("activation", dict(out=c16[:], in_=c16[:], func=AF.Exp), r=[cdB], w=[cdB])
                    V("tensor_reduce", dict(out=Z, in_=c16[:], axis=AX.X, op=ALU.add), r=[cdB], w=[rB[p]])
                    A("activation", dict(out=lnZ, in_=Z, func=AF.Ln), r=[rB[p]], w=[rB[p]])
                    V("tensor_tensor", dict(out=adj, in0=negm, in1=lnZ, op=ALU.subtract), r=[rB[p]], w=[rB[p]])
                    yield

                def gen_X2(blk):
                    p = blk % 2
                    thr, mx, negm, Z, lnZ, adj = [sm8[p][:, 8 * i:8 * i + 8] for i in range(6)]
                    for h in range(8):
                        k = h % 2
                        hs = slice(16 * h, 16 * h + 16)
                        v1b = top[p][:, h, 0, :].unsqueeze(2).to_broadcast([128, 16, 128])
                        V("tensor_tensor", dict(out=smm[k][:], in0=v1b,
                                                in1=sall[p][:, h, 1, :].unsqueeze(1).to_broadcast([128, 16, 128]),
                                                op=ALU.add), r=[topB[p], saB[p]], w=[smB[k]])
                        V("tensor_scalar", dict(out=msk[k][:], in0=smm[k][:], scalar1=thr[:, h:h + 1], scalar2=None,
                                                op0=ALU.is_ge), r=[smB[k], rB[p]], w=[mkB[k]])
                        A("activation", dict(out=ex[k][:], in_=smm[k][:], func=AF.Exp, bias=adj[:, h:h + 1]),
                          r=[smB[k], rB[p]], w=[exB[k]])
                        G("tensor_tensor", dict(out=R[:, hs, :], in0=ex[k][:], in1=msk[k][:], op=ALU.mult),
                          r=[exB[k], mkB[k]], w=[RB])
                        V("tensor_tensor", dict(out=OH[:, hs, :], in0=v1b,
                                                in1=sall[p][:, h, 0, :].unsqueeze(1).to_broadcast([128, 16, 128]),
                                                op=ALU.is_equal), r=[topB[p], saB[p]], w=[OHB])
                        yield

                def gen_Y(blk):
                    t0 = blk * 128
                    for th in range(2):
                        ts_ = slice(64 * th, 64 * th + 64)
                        Gs, GsB = Gs2[th], Gs2B[th]
                        pend = None
                        nev = 0
                        for (src, srcB, dst, dstB) in ((R, RB, RT, RTB), (OH, OHB, PT, PTB)):
                            for i8 in range(16):
                                kk = rr["tp"] % 3
                                rr["tp"] += 1
                                for ii in range(8):
                                    i = 8 * i8 + ii
                                    P.opk("tensor", "transpose", dict(out=tpp[kk][:, ii * 64:(ii + 1) * 64],
                                                                      in_=src[ts_, :, i], identity=idb[ts_, ts_]),
                                          [srcB, cB], [tpB[kk]])
                                if pend is not None:
                                    pend()
                                def ev(kk=kk, i8=i8, dst=dst, dstB=dstB, n=nev):
                                    f = A
                                    f("copy",
                                      dict(out=dst[:, 8 * i8:8 * i8 + 8, :],
                                           in_=tpp[kk][:, 0:512].rearrange("p (i t) -> p i t", t=64)),
                                      r=[tpB[kk]], w=[dstB])
                                pend = ev
                                nev += 1
                                yield
                        pend()
                        pend = None
                        for t4 in range(16):
                            kk = rr["pg"] % 3
                            rr["pg"] += 1
                            for tt in range(4):
                                t = 4 * t4 + tt
                                P.mm(pg[kk][:, tt * 128:(tt + 1) * 128], RT[:, :, t], PT[:, :, t], True, True,
                                     reads=[RTB, PTB], writes=[pgB[kk]])
                            if pend is not None:
                                pend()
                            def ev2(kk=kk, t4=t4, Gs=Gs, GsB=GsB):
                                dsto = Gs[:, :, 4 * t4:4 * t4 + 4].rearrange("p i t -> p t i")
                                srci = pg[kk][:].rearrange("p (t i) -> p t i", t=4)
                                A("copy", dict(out=dsto, in_=srci), r=[pgB[kk]], w=[GsB])
                            pend = ev2
                            yield
                        pend()
                        tg = t0 + 64 * th
                        for q in range(8):
                            P.dma("sync", Gd[16 * q:16 * q + 16, :, tg:tg + 64].rearrange("a b t -> b a t"),
                                  Gs[:, 16 * q:16 * q + 16, :], reads=[GsB])
                        yield

                def merge2(ga, gb, wa=1):
                    live = [(g, w) for g, w in ((ga, wa), (gb, 1)) if g is not None]
                    while live:
                        for (g, w) in list(live):
                            for _ in range(w):
                                try:
                                    next(g)
                                except StopIteration:
                                    live.remove((g, w))
                                    break

                merge2(gen_X(0), None)
                merge2(gen_X2(0), None)
                for blk in range(NBL):
                    merge2(gen_Y(blk), gen_X(blk + 1) if blk + 1 < NBL else None, wa=4)
                    if blk + 1 < NBL:
                        merge2(gen_X2(blk + 1), None)
                P.barrier()
                P.emit()

        if 4 in stages:
            with ExitStack() as es:
                sb, pt = mk(es)
                GE = 4
                h2t = sb("h2t", [128, NCH, 512], BF16)
                h2B = Buf()
                ust = [sb(f"ust{k}", [128, NCH, 128]) for k in range(2)]
                ustB = [Buf(), Buf()]
                ub = [sb(f"ub{k}", [128, NCH, 128], BF16) for k in range(3)]
                ubB = [Buf() for k in range(3)]
                vst = [sb(f"vst{k}", [128, D]) for k in range(2)]
                vstB = [Buf(), Buf()]
                vb = [sb(f"vb{k}", [128, D], BF16) for k in range(2 * GE)]
                vbB = [Buf() for k in range(2 * GE)]
                gch = [sb(f"gch{k}", [128, 512], BF16) for k in range(3)]
                gchB = [Buf() for k in range(3)]
                ge = [sb(f"ge{k}", [128, 512]) for k in range(2)]
                geB = [Buf(), Buf()]
                Wt = [sb(f"Wt{k}", [128, 512], BF16) for k in range(2 * GE)]
                WtB = [Buf() for k in range(2 * GE)]
                oacc = sb("oacc", [128, NCH, 512])
                oaB = Buf()
                xc = [sb(f"xc{k}", [128, 512]) for k in range(2)]
                xcB = [Buf(), Buf()]
                sq = [sb(f"sq{k}", [128, 512], BF16) for k in range(2)]
                sqB = [Buf(), Buf()]
                rs = sb("rs", [128, 512])
                rsB = Buf()
                ot = [sb(f"ot{k}", [128, 512]) for k in range(2)]
                otB = [Buf(), Buf()]
                pa = [pt(f"pa{k}", [128, 512]) for k in range(3)]
                paB = [Buf() for k in range(3)]
                pacc = [pt(f"pacc{k}", [128, 512]) for k in range(3)]
                paccB = [Buf() for k in range(3)]
                pn = [0, 0]
                ln2b = ln2t[:, :].unsqueeze(2).to_broadcast([128, NCH, 128])
                ubdB = [Buf() for _ in range(128)]
                vbdB = [Buf() for _ in range(128)]
                pend_grp = None

                def emit_group(base, first):
                    for dc in range(NCH):
                        kq = pn[1] % 3
                        pn[1] += 1
                        for gi in range(GE):
                            P.mm(pacc[kq][:], vb[base + gi][:, dc * 128:(dc + 1) * 128], Wt[base + gi][:],
                                 gi == 0, gi == GE - 1, reads=[vbB[base + gi], WtB[base + gi]],
                                 writes=[paccB[kq]])
                        if first:
                            V("tensor_copy", dict(out=oacc[:, dc, :], in_=pacc[kq][:]), r=[paccB[kq]], w=[oaB])
                        else:
                            V("tensor_tensor", dict(out=oacc[:, dc, :], in0=oacc[:, dc, :], in1=pacc[kq][:],
                                                    op=ALU.add), r=[paccB[kq]], w=[oaB])
                for j in range(NTL):
                    cols = slice(j * 512, (j + 1) * 512)
                    P.dma("sync", h2t[:], h2v[:, :, cols], writes=[h2B])
                    for i1 in range(128):
                        s2 = i1 % 2
                        s3 = i1 % 3
                        sv = i1 % (2 * GE)
                        es_ = slice(i1 * 128, (i1 + 1) * 128)
                        P.dma("sync", gch[s3][:], Gd[i1, :, cols], writes=[gchB[s3]])
                        if j == 0:
                            P.dma("sync", ust[s2][:].rearrange("p c e -> p (c e)"), uTd[i1, :, :], writes=[ustB[s2]])
                            P.dma("sync", vst[s2][:], vd[es_, :], writes=[vstB[s2]])
                            G("tensor_tensor", dict(out=ub[s3][:], in0=ust[s2][:], in1=ln2b, op=ALU.mult),
                              r=[ustB[s2], cB], w=[ubB[s3]])
                            if i1 % 2 == 0:
                                V("tensor_copy", dict(out=vb[sv][:], in_=vst[s2][:]), r=[vstB[s2]], w=[vbB[sv]])
                            else:
                                A("copy", dict(out=vb[sv][:], in_=vst[s2][:]), r=[vstB[s2]], w=[vbB[sv]])
                            if NTL > 1:
                                P.dma("scalar", ubd[i1, :, :], ub[s3][:].rearrange("p c e -> p (c e)"),
                                      reads=[ubB[s3]], writes=[ubdB[i1]])
                                P.dma("scalar", vbd[i1, :, :], vb[sv][:], reads=[vbB[sv]], writes=[vbdB[i1]])
                        else:
                            P.dma("sync", ub[s3][:].rearrange("p c e -> p (c e)"), ubd[i1, :, :],
                                  reads=[ubdB[i1]], writes=[ubB[s3]])
                            P.dma("sync", vb[sv][:], vbd[i1, :, :], reads=[vbdB[i1]], writes=[vbB[sv]])
                        kp = pn[0] % 3
                        pn[0] += 1
                        for c in range(NCH):
                            P.mm(pa[kp][:], ub[s3][:, c, :], h2t[:, c, :], c == 0, c == NCH - 1,
                                 reads=[ubB[s3], h2B], writes=[paB[kp]])
                        if pend_grp is not None:
                            emit_group(*pend_grp)
                            pend_grp = None
                        A("activation", dict(out=ge[s2][:], in_=pa[kp][:], func=AF.Gelu), r=[paB[kp]], w=[geB[s2]])
                        V("tensor_tensor", dict(out=Wt[sv][:], in0=ge[s2][:], in1=gch[s3][:], op=ALU.mult),
                          r=[geB[s2], gchB[s3]], w=[WtB[sv]])
                        if i1 % GE == GE - 1:
                            pend_grp = (sv - (GE - 1), i1 == GE - 1)
                    if pend_grp is not None:
                        emit_group(*pend_grp)
                        pend_grp = None
                    for dc in range(NCH):
                        k = dc % 2
                        P.dma("sync", xc[k][:], x1v[:, dc, cols], writes=[xcB[k]])
                        V("tensor_tensor", dict(out=oacc[:, dc, :], in0=oacc[:, dc, :], in1=xc[k][:], op=ALU.add),
                          r=[xcB[k]], w=[oaB])
                    kq = pn[1] % 3
                    pn[1] += 1
                    rms_bcast(pacc[kq], paccB[kq], [oacc[:, c, :] for c in range(NCH)], oaB, sq, sqB, NCH, 512)
                    A("activation", dict(out=rs[:], in_=pacc[kq][:], func=AF.Ln, scale=1.0 / D, bias=1e-6),
                      r=[paccB[kq]], w=[rsB])
                    A("activation", dict(out=rs[:], in_=rs[:], func=AF.Exp, scale=-0.5), r=[rsB], w=[rsB])
                    for dc in range(NCH):
                        k = dc % 2
                        V("scalar_tensor_tensor", dict(out=ot[k][:], in0=oacc[:, dc, :], scalar=lnft[:, dc:dc + 1],
                                                       in1=rs[:], op0=ALU.mult, op1=ALU.mult),
                          r=[oaB, rsB, cB], w=[otB[k]])
                        P.dma("sync", outv[:, dc, cols], ot[k][:], reads=[otB[k]])
                P.barrier()
                P.emit()
        P.finish()
        P.emit()
    return nc


def prep_p1(inp, c, T):
    g = c // 4
    w_in = inp["w_in"]
    cols = np.concatenate([
        np.arange(0 + 128 * c, 128 * c + 128),
        np.arange(1024 + 128 * c, 1024 + 128 * c + 128),
        np.arange(2048 + 128 * g, 2048 + 128 * g + 128),
        np.arange(2304 + 128 * g, 2304 + 128 * g + 128),
        np.arange(2576 + 128 * c, 2576 + 128 * c + 128),
        np.arange(3600 + 128 * c, 3600 + 128 * c + 128),
        np.arange(4624 + 128 * c, 4624 + 128 * c + 128),
        np.arange(2560 + 2 * c, 2560 + 2 * c + 2),
        np.arange(5648 + 2 * c, 5648 + 2 * c + 2),
    ])
    wf = np.ascontiguousarray(w_in[:, cols])
    ln1 = np.ascontiguousarray(inp["ln1_w"].reshape(16, 128).T)
    chans = [np.arange(128 * c, 128 * c + 128), np.arange(1024 + 128 * g, 1024 + 128 * g + 128),
             np.arange(1280 + 128 * g, 1280 + 128 * g + 128)]
    cw = np.zeros((128, 12), np.float32)
    cb = np.zeros((128, 3), np.float32)
    for gi, ch in enumerate(chans):
        cw[:, 4 * gi:4 * gi + 4] = inp["conv_w"][:, ch].T
        cb[:, gi] = inp["conv_b"][ch]
    hp = np.zeros((128, 8), np.float32)
    for k, name in enumerate(["dt_bias", "a_log", "d_skip", "fox_f_bias"]):
        hp[:, 2 * k] = inp[name][2 * c]
        hp[:, 2 * k + 1] = inp[name][2 * c + 1]
    return {"wf": wf, "ln1": ln1, "cw": cw, "cb": cb, "hp": hp}


SEQ = 16384
NCORES = 8


def kernel(x, ln1_w, w_in, conv_w, conv_b, dt_bias, a_log, d_skip, ssd_norm_w,
           fox_f_bias, w_out, ln2_w, peer_wq, peer_k1, peer_k2, peer_u, peer_v, lnf_w):
    f32 = lambda a: np.ascontiguousarray(np.asarray(a, dtype=np.float32))
    inp = {"w_in": f32(w_in), "ln1_w": f32(ln1_w), "conv_w": f32(conv_w), "conv_b": f32(conv_b),
           "dt_bias": f32(dt_bias), "a_log": f32(a_log), "d_skip": f32(d_skip), "fox_f_bias": f32(fox_f_bias)}
    T = SEQ
    xT = np.ascontiguousarray(f32(x)[0].T)
    nc1 = build_p1(T)
    in_maps = []
    for c in range(NCORES):
        d = prep_p1(inp, c, T)
        d["xT"] = xT
        in_maps.append(d)
    res1 = run_bass_kernel_spmd(nc1, in_maps, core_ids=list(range(NCORES)))
    yT = np.empty((2048, T), np.float32)
    for c in range(NCORES):
        r = res1.results[c]["yT"]
        yT[128 * c:128 * c + 128] = r[0:128]
        yT[1024 + 128 * c:1024 + 128 * c + 128] = r[128:256]
    TT = T // NCORES
    nc2 = build_p2(TT)
    cl = lambda a: np.ascontiguousarray(f32(a).reshape(16, 128).T)
    shared = {"wo": f32(w_out), "wq": f32(peer_wq),
              "snw": np.ascontiguousarray(f32(ssd_norm_w).reshape(8, 128).T), "ln2": cl(ln2_w), "lnf": cl(lnf_w),
              "k1T": np.ascontiguousarray(f32(peer_k1).transpose(2, 0, 1)),
              "k2T": np.ascontiguousarray(f32(peer_k2).transpose(2, 0, 1)),
              "uT": np.ascontiguousarray(f32(peer_u).reshape(128, 128, 16, 128).transpose(0, 3, 2, 1)).reshape(128, 128, 2048),
              "vv": f32(peer_v)}
    in_maps = []
    for c in range(NCORES):
        d = dict(shared)
        d["yTin"] = np.ascontiguousarray(yT[:, c * TT:(c + 1) * TT])
        d["xTin"] = np.ascontiguousarray(xT[:, c * TT:(c + 1) * TT])
        in_maps.append(d)
    res2 = run_bass_kernel_spmd(nc2, in_maps, core_ids=list(range(NCORES)))
    out = np.empty((1, T, 2048), np.float32)
    for c in range(NCORES):
        out[0, c * TT:(c + 1) * TT, :] = res2.results[c]["outT"].T
    return out
```

```python
from concourse.bass_utils import run_bass_kernel_spmd

import numpy as np
import concourse.bass as bass
import concourse.mybir as mybir
from contextlib import ExitStack

F32 = mybir.dt.float32
BF16 = mybir.dt.bfloat16
AF = mybir.ActivationFunctionType
ALU = mybir.AluOpType
AX = mybir.AxisListType


class Buf:
    __slots__ = ("w", "r", "name")

    def __init__(self, name=""):
        self.w = None
        self.r = []
        self.name = name


class Prog:
    ENG = ["sync", "scalar", "vector", "gpsimd", "tensor"]

    def __init__(self, nc, es, n_dma_sems=24):
        self.nc = nc
        self.es = es
        self.ops = {e: [] for e in self.ENG}
        self.sem = {e: es.enter_context(nc.semaphore("s_" + e)) for e in self.ENG}
        self.cnt = {e: 0 for e in self.ENG}
        self.dsem = [es.enter_context(nc.semaphore(f"dq{i}")) for i in range(n_dma_sems)]
        self.dcnt = [0] * n_dma_sems
        self.dnext = 0
        self.waited = {e: {} for e in self.ENG}
        self.nwaits = 0

    def _semof(self, key):
        return self.sem[key[1]] if key[0] == "e" else self.dsem[key[1]]

    def _issue(self, eng, fn, reads, writes, key, inc, extra_deps=()):
        deps = set(extra_deps)
        for b in reads:
            if b.w is not None:
                deps.add(b.w)
        for b in writes:
            if b.w is not None:
                deps.add(b.w)
            deps.update(b.r)
        waits = []
        best = {}
        for (k, v) in deps:
            if k == ("e", "tensor") and key == ("e", "tensor"):
                continue
            if best.get(k, 0) < v:
                best[k] = v
        for k, v in best.items():
            if self.waited[eng].get(k, 0) >= v:
                continue
            self.waited[eng][k] = v
            waits.append((self._semof(k), v))
        self.nwaits += len(waits)
        if key[0] == "e":
            self.cnt[eng] += inc
            val = self.cnt[eng]
        else:
            self.dcnt[key[1]] += inc
            val = self.dcnt[key[1]]
        self.ops[eng].append((waits, fn, (self._semof(key), inc)))
        tok = (key, val)
        for b in reads:
            b.r.append(tok)
        for b in writes:
            b.w = tok
            b.r = []
        return tok

    def op(self, eng, fn, reads=(), writes=()):
        return self._issue(eng, fn, list(reads), list(writes), ("e", eng), 1)

    def opk(self, eng, name, kw, reads=(), writes=()):
        return self.op(eng, lambda e: getattr(e, name)(**kw), reads, writes)

    def dma(self, eng, out, in_, reads=(), writes=(), **kw):
        i = self.dnext % len(self.dsem)
        self.dnext += 1
        key = ("d", i)
        extra = []
        if self.dcnt[i] > 0:
            extra.append((key, self.dcnt[i]))
        return self._issue(eng, lambda e: e.dma_start(out=out, in_=in_, **kw),
                           list(reads), list(writes), key, 16, extra)

    def mm(self, out, lhsT, rhs, start, stop, reads=(), writes=()):
        return self.op("tensor", lambda e: e.matmul(out, lhsT, rhs, start=start, stop=stop),
                       reads, writes)

    def act(self, out, in_, func, reads=(), writes=(), **kw):
        return self.op("scalar", lambda e: e.activation(out=out, in_=in_, func=func, **kw),
                       reads, writes)

    def finish(self, bufs=()):
        deps = []
        for i, c in enumerate(self.dcnt):
            if c > 0:
                deps.append((("d", i), c))
        for e in self.ENG:
            if e != "sync" and self.cnt[e] > 0:
                deps.append((("e", e), self.cnt[e]))
        waits = []
        for k, v in deps:
            if self.waited["sync"].get(k, 0) >= v:
                continue
            self.waited["sync"][k] = v
            waits.append((self._semof(k), v))
        self.ops["sync"].append((waits, None, None))

    def barrier(self):
        deps = []
        for i, c in enumerate(self.dcnt):
            if c > 0:
                deps.append((("d", i), c))
        for e in self.ENG:
            if self.cnt[e] > 0:
                deps.append((("e", e), self.cnt[e]))
        for e in self.ENG:
            waits = []
            for k, v in deps:
                if k == ("e", e) and e == "tensor":
                    continue
                if self.waited[e].get(k, 0) >= v:
                    continue
                self.waited[e][k] = v
                waits.append((self._semof(k), v))
            self.ops[e].append((waits, None, None))

    def emit(self):
        with self.nc.Block() as block:
            for e in self.ENG:
                ops = self.ops[e]

                def body(eng, ops=ops):
                    for (waits, fn, inc) in ops:
                        for (s, v) in waits:
                            eng.wait_ge(s, v)
                        if fn is None:
                            continue
                        ins = fn(eng)
                        ins.then_inc(inc[0], inc[1])

                getattr(block, e)(body)
        self.ops = {e: [] for e in self.ENG}


D = 2048
NCH = 16
FM = [("z", 0, 128), ("xs", 128, 128), ("B", 256, 128), ("C", 384, 128),
      ("q", 512, 128), ("k", 640, 128)]
TM0 = 768
NW = 900


def build_p1(T):
    NT = T // 512
    NB = T // 128
    nc = bass.Bass("TRN2", target_bir_lowering=False)
    xT = nc.dram_tensor("xT", [D, T], F32, kind="ExternalInput").ap()
    wf = nc.dram_tensor("wf", [D, NW], F32, kind="ExternalInput").ap()
    ln1 = nc.dram_tensor("ln1", [128, NCH], F32, kind="ExternalInput").ap()
    cwd = nc.dram_tensor("cw", [128, 12], F32, kind="ExternalInput").ap()
    cbd = nc.dram_tensor("cb", [128, 3], F32, kind="ExternalInput").ap()
    hpd = nc.dram_tensor("hp", [128, 8], F32, kind="ExternalInput").ap()
    yT = nc.dram_tensor("yT", [256, T], F32, kind="ExternalOutput").ap()
    xTv = xT.rearrange("(c p) t -> p c t", p=128)

    with ExitStack() as es:
        P = Prog(nc, es)

        def sb(name, shape, dt=F32):
            return es.enter_context(nc.sbuf_tensor(name, shape, dt))

        def pt(name, shape, dt=F32):
            return es.enter_context(nc.psum_tensor(name, shape, dt))

        V = lambda f, kw, r=(), w=(): P.opk("vector", f, kw, r, w)
        G = lambda f, kw, r=(), w=(): P.opk("gpsimd", f, kw, r, w)
        A = lambda f, kw, r=(), w=(): P.opk("scalar", f, kw, r, w)

        cB = Buf("const")
        U = sb("U", [128, 128])
        SL = sb("SL", [128, 128])
        ones = sb("ones", [128, 128])
        onesb = sb("onesb", [128, 128], BF16)
        idb = sb("idb", [128, 128], BF16)
        idf = sb("idf", [128, 128])
        G("memset", dict(ap=U[:], constant=1.0), w=[cB])
        G("affine_select", dict(out=U[:], in_=U[:], pattern=[[1, 128]], compare_op=ALU.is_ge,
                                    fill=0.0, base=0, channel_multiplier=-1), w=[cB])
        G("memset", dict(ap=SL[:], constant=1.0), w=[cB])
        G("affine_select", dict(out=SL[:], in_=SL[:], pattern=[[-1, 128]], compare_op=ALU.is_gt,
                                    fill=0.0, base=0, channel_multiplier=1), w=[cB])
        G("memset", dict(ap=ones[:], constant=1.0), w=[cB])
        G("memset", dict(ap=onesb[:], constant=1.0), w=[cB])
        G("memset", dict(ap=idf[:], constant=1.0), w=[cB])
        G("affine_select", dict(out=idf[:], in_=idf[:], pattern=[[1, 128]], compare_op=ALU.is_equal,
                                    fill=0.0, base=0, channel_multiplier=-1), w=[cB])
        G("tensor_copy", dict(out=idb[:], in_=idf[:]), r=[cB], w=[cB])

        ln1t = sb("ln1t", [128, NCH])
        cw = sb("cwt", [128, 12])
        cb = sb("cbt", [128, 3])
        hp = sb("hpt", [128, 8])
        P.dma("sync", ln1t[:], ln1, writes=[cB])
        P.dma("sync", cw[:], cwd, writes=[cB])
        P.dma("sync", cb[:], cbd, writes=[cB])
        P.dma("sync", hp[:], hpd, writes=[cB])
        hq = sb("hq", [128, 4])
        A("activation", dict(out=hq[:, 0:2], in_=hp[:, 2:4], func=AF.Exp), r=[cB], w=[cB])
        V("tensor_scalar", dict(out=hq[:, 0:2], in0=hq[:, 0:2], scalar1=-1.0, scalar2=None, op0=ALU.mult), r=[cB], w=[cB])
        V("tensor_scalar", dict(out=hq[:, 2:4], in0=hp[:, 6:8], scalar1=-1.0, scalar2=None, op0=ALU.mult), r=[cB], w=[cB])

        W = sb("W", [128, NCH, NW], BF16)
        wB = Buf("W")
        wst1 = sb("wst", [128, NW])
        wst = [wst1] * 2
        wstB = [Buf()] * 2
        for c in range(NCH):
            k = c % 2
            P.dma("sync", wst[k][:], wf[c * 128:(c + 1) * 128, :], writes=[wstB[k]])
            V("tensor_scalar", dict(out=W[:, c, :], in0=wst[k][:], scalar1=ln1t[:, c:c + 1],
                                                 scalar2=None, op0=ALU.mult), r=[wstB[k], cB], w=[wB])

        k2 = sb("k2", [128, T], BF16)
        kB_ = [Buf() for _ in range(NT)]
        Vaug = sb("Vaug", [128, NB, 2, 68], BF16)
        vB_ = [Buf() for _ in range(NT)]
        Ccol = sb("Ccol", [128, 2, NB])
        ccB = [Buf(), Buf()]
        carry = sb("carry", [128, 2])
        pre = [sb(f"pre{h}", [128, 5]) for h in range(2)]
        pad = [sb(f"pad{h}", [128, 4], BF16) for h in range(2)]
        padB = [Buf(), Buf()]
        hTf = [sb(f"hTf{h}", [128, 64]) for h in range(2)]
        hTb = [sb(f"hTb{h}", [128, 64], BF16) for h in range(2)]
        hfB = [Buf(), Buf()]
        hbB = [Buf(), Buf()]
        xbc = [sb(f"xbc{g}", [128, 515]) for g in range(3)]
        xbcB = [Buf() for g in range(3)]
        for h in range(2):
            G("memset", dict(ap=pre[h][:], constant=0.0), w=[padB[h]])
            G("memset", dict(ap=pad[h][:], constant=0.0), w=[padB[h]])
            G("memset", dict(ap=hTf[h][:], constant=0.0), w=[hfB[h]])
            G("memset", dict(ap=hTb[h][:], constant=0.0), w=[hbB[h]])
        G("memset", dict(ap=Vaug[:, :, :, 64:65], constant=1.0), w=vB_)
        G("memset", dict(ap=carry[:], constant=0.0), w=ccB)
        for g in range(3):
            G("memset", dict(ap=xbc[g][:, 0:3], constant=0.0), w=[xbcB[g]])

        xts = [sb(f"xt{k}", [128, NCH, 256]) for k in range(2)]
        xtBs = [Buf(), Buf()]
        sq = [sb(f"sq{k}", [128, 512], BF16) for k in range(2)]
        sqB = [Buf(), Buf()]
        rstd = sb("rstd", [128, 512])
        lnv = rstd
        rsB = Buf()
        hT = sb("hT", [128, NCH, 512], BF16)
        hB = Buf()
        cacc1 = sb("cacc", [128, 512])
        cacc = [cacc1] * 3
        caB = [Buf()] * 3
        zs = [sb(f"zs{p}", [128, 512]) for p in range(2)]
        zsB = [Buf(), Buf()]
        xcv = [[sb(f"xcv{p}{g}", [128, 512], BF16) for g in range(3)] for p in range(2)]
        xcB = [[Buf() for g in range(3)] for p in range(2)]
        q2 = [[sb(f"q2{p}{h}", [128, 512], BF16) for h in range(2)] for p in range(2)]
        q2B = [Buf(), Buf()]
        drow = [[sb(f"drow{p}{h}", [128, 512], BF16) for h in range(2)] for p in range(2)]
        drB = [[Buf(), Buf()] for p in range(2)]
        for p_ in range(2):
            for h_ in range(2):
                G("memset", dict(ap=q2[p_][h_][:], constant=0.0), w=[q2B[p_]])
                G("memset", dict(ap=drow[p_][h_][:], constant=0.0), w=[drB[p_][h_]])
        dtc = [sb(f"dtc{p}", [128, 4, 2]) for p in range(2)]
        acol = [sb(f"acol{p}", [128, 4, 2]) for p in range(2)]
        colB = [Buf(), Buf()]
        biasT = [[sb(f"biasT{p}{h}", [128, NB]) for h in range(2)] for p in range(2)]
        biB = [[Buf(), Buf()] for p in range(2)]
        sm = sb("sm", [128, 4, 4])
        smB = Buf()
        e1 = sb("e1", [128, 4, 4])
        LF = sb("LF", [128, 2, 4])
        lfB = Buf()
        ptile = [sb(f"ptile{k}", [128, 512], BF16) for k in range(3)]
        ptB = [Buf() for k in range(3)]
        oT = sb("oT", [65, 512])
        oTB = Buf()
        rec = sb("rec", [64, 512])
        recB = Buf()
        yfx = rec
        yfxB = recB
        ygt = sb("ygt", [128, 512])
        ygB = Buf()
        xs_tok = [sb(f"xstok{k}", [128, 128]) for k in range(2)]
        xdt = [sb(f"xdt{k}", [128, 128], BF16) for k in range(2)]
        Btok = [sb(f"Btok{k}", [128, 128], BF16) for k in range(2)]
        tkB = [Buf(), Buf()]
        cbm = [sb(f"cbm{k}", [128, 128]) for k in range(2)]
        cbmB = [Buf(), Buf()]
        Wa = [[sb(f"Wa{k}{h}", [128, 256]) for h in range(2)] for k in range(2)]
        WaB = [[Buf(), Buf()] for k in range(2)]
        E = [[sb(f"E{k}{h}", [128, 256]) for h in range(2)] for k in range(2)]
        EB = [[Buf(), Buf()] for k in range(2)]
        MT = [[sb(f"MT{k}{h}", [128, 128], BF16) for h in range(2)] for k in range(2)]
        Cp = [[sb(f"Cp{k}{h}", [128, 128], BF16) for h in range(2)] for k in range(2)]
        xdtd = [[sb(f"xdtd{k}{h}", [128, 64], BF16) for h in range(2)] for k in range(2)]
        mB = [[Buf(), Buf()] for k in range(2)]
        ytok = [sb(f"ytok{k}", [128, 128]) for k in range(2)]
        ytB = [Buf(), Buf()]

        bigL = [pt(f"bigL{k}", [128, 512]) for k in range(2)]
        bigLB = [Buf() for k in range(2)]
        bigF = [pt(f"bigF{k}", [128, 512]) for k in range(2)]
        bigFB = [Buf() for k in range(2)]
        cnt = {"L": 0, "F": 0, "pt": 0}

        def nextL():
            k = cnt["L"] % 2
            cnt["L"] += 1
            return bigL[k], bigLB[k]

        def nextF():
            k = cnt["F"] % 2
            cnt["F"] += 1
            return bigF[k], bigFB[k]
        po = pt("po", [128, 512])
        poB = Buf()
        tp = pt("tp", [128, 256], BF16)
        tpB = Buf()
        ssd1 = pt("ssd1", [128, 512])
        pcb = ssd1[:, 0:128]
        pcbB = Buf()
        py = ssd1[:, 128:256]
        pyB = pcbB
        pst = ssd1[:, 256:384]
        pstB = pcbB
        pyt = ssd1[:, 384:512]
        pytB = pcbB
        psegt = pt("psegt", [128, 512])
        pseg = [psegt[:, 256 * h:256 * h + 256] for h in range(2)]
        psegB = [Buf()] * 2

        def gen_L(i):
            p = i % 2
            cols = slice(i * 512, (i + 1) * 512)
            for half in range(2):
                h0 = half * 256
                xt, xtB = xts[half], xtBs[half]
                for q in range(4):
                    P.dma("sync", xt[:, 4 * q:4 * q + 4, :],
                          xTv[:, 4 * q:4 * q + 4, i * 512 + h0:i * 512 + h0 + 256], writes=[xtB])
                pss, pssB = nextL()
                for c in range(NCH):
                    k = c % 2
                    G("tensor_tensor", dict(out=sq[k][:, 0:256], in0=xt[:, c, :], in1=xt[:, c, :], op=ALU.mult),
                      r=[xtB], w=[sqB[k]])
                    P.mm(pss[:, 0:256], onesb[:], sq[k][:, 0:256], c == 0, c == NCH - 1,
                         reads=[sqB[k], cB], writes=[pssB])
                yield
                A("activation", dict(out=lnv[:, 0:256], in_=pss[:, 0:256], func=AF.Ln, scale=1.0 / D, bias=1e-6),
                  r=[pssB], w=[rsB])
                A("activation", dict(out=rstd[:, 0:256], in_=lnv[:, 0:256], func=AF.Exp, scale=-0.5),
                  r=[rsB], w=[rsB])
                for c in range(NCH):
                    V("tensor_tensor", dict(out=hT[:, c, h0:h0 + 256], in0=xt[:, c, :], in1=rstd[:, 0:256],
                                            op=ALU.mult), r=[xtB, rsB], w=[hB])
                yield
            def fm_post(name, ps, psB):
                if name == "z":
                    A("activation", dict(out=zs[p][:], in_=ps[:], func=AF.Silu), r=[psB], w=[zsB[p]])
                elif name in ("xs", "B", "C"):
                    g = ("xs", "B", "C").index(name)
                    A("copy", dict(out=xbc[g][:, 3:515], in_=ps[:]), r=[psB], w=[xbcB[g]])
                elif name == "q":
                    A("mul", dict(out=q2[p][0][0:64, :], in_=ps[0:64, :], mul=0.125), r=[psB], w=[q2B[p]])
                    A("mul", dict(out=q2[p][1][64:128, :], in_=ps[64:128, :], mul=0.125), r=[psB], w=[q2B[p]])
                else:
                    V("tensor_copy", dict(out=k2[:, cols], in_=ps[:]), r=[psB], w=[kB_[i]])

            def fm_conv(name):
                if name in ("xs", "B", "C"):
                    g = ("xs", "B", "C").index(name)
                    V("tensor_scalar", dict(out=cacc[g][:], in0=xbc[g][:, 0:512],
                                            scalar1=cw[:, 4 * g:4 * g + 1], scalar2=None, op0=ALU.mult),
                      r=[xbcB[g], cB], w=[caB[g]])
                    for kk in range(1, 4):
                        V("scalar_tensor_tensor", dict(
                            out=cacc[g][:], in0=xbc[g][:, kk:kk + 512], scalar=cw[:, 4 * g + kk:4 * g + kk + 1],
                            in1=cacc[g][:], op0=ALU.mult, op1=ALU.add), r=[xbcB[g], cB], w=[caB[g]])
                    V("tensor_copy", dict(out=xbc[g][:, 0:3], in_=xbc[g][:, 512:515]),
                      r=[xbcB[g]], w=[xbcB[g]])

            def fm_silu(name):
                if name in ("xs", "B", "C"):
                    g = ("xs", "B", "C").index(name)
                    A("activation", dict(out=xcv[p][g][:], in_=cacc[g][:], func=AF.Silu,
                                         bias=cb[:, g:g + 1]), r=[caB[g], cB], w=[xcB[p][g]])

            pipe = []
            for (name, c0, wd) in FM + [(None, 0, 0)] * 3:
                nxt = []
                for (stg_, nm, ps_, psB_) in pipe:
                    if stg_ == 1:
                        fm_post(nm, ps_, psB_)
                        nxt.append((2, nm, None, None))
                    elif stg_ == 2:
                        fm_conv(nm)
                        nxt.append((3, nm, None, None))
                    else:
                        fm_silu(nm)
                pipe = nxt
                if name is not None:
                    ps, psB = nextL()
                    for c in range(NCH):
                        P.mm(ps[0:wd, :], W[:, c, c0:c0 + wd], hT[:, c, :], c == 0, c == NCH - 1,
                             reads=[wB, hB], writes=[psB])
                    pipe.append((1, name, ps, psB))
                yield
            pend_tm = None
            for b in range(5):
                if b < 4:
                    blk = 4 * i + b
                    ps, psB = nextL()
                    for c in range(NCH):
                        P.mm(ps[:, 0:132], hT[:, c, b * 128:(b + 1) * 128], W[:, c, TM0:TM0 + 132],
                             c == 0, c == NCH - 1, reads=[wB, hB], writes=[psB])
                if pend_tm is not None:
                    (pb, pblk, pps, ppsB) = pend_tm
                    V("tensor_copy", dict(out=Vaug[:, pblk, :, 0:64],
                                          in_=pps[:, 0:128].rearrange("p (h d) -> p h d", h=2)),
                      r=[ppsB], w=[vB_[i]])
                    V("tensor_copy", dict(out=sm[:, pb, :], in_=pps[:, 128:132]), r=[ppsB], w=[smB])
                pend_tm = (b, blk, ps, psB) if b < 4 else None
                yield
            for h in range(2):
                A("activation", dict(out=e1[:, :, h], in_=sm[:, :, h], func=AF.Exp,
                                     bias=hp[:, h:h + 1]), r=[smB, cB], w=[lfB])
                A("activation", dict(out=e1[:, :, 2 + h], in_=sm[:, :, 2 + h], func=AF.Exp,
                                     scale=-1.0, bias=hq[:, 2 + h:3 + h]), r=[smB, cB], w=[lfB])
            yield
            A("activation", dict(out=e1[:], in_=e1[:], func=AF.Ln, bias=1.0), r=[lfB], w=[lfB])
            yield
            V("tensor_copy", dict(out=dtc[p][:], in_=e1[:, :, 0:2]), r=[lfB], w=[colB[p]])
            for h in range(2):
                V("tensor_scalar", dict(out=acol[p][:, :, h], in0=e1[:, :, h], scalar1=hq[:, h:h + 1],
                                        scalar2=None, op0=ALU.mult), r=[lfB, cB], w=[colB[p]])
                V("tensor_scalar", dict(out=LF[:, h, :], in0=e1[:, :, 2 + h], scalar1=-1.0,
                                        scalar2=None, op0=ALU.mult), r=[lfB], w=[lfB])
            nkb = 4 * i + 4
            for h in range(2):
                pcf, pcB_ = nextL()
                pc = pcf[:, 0:5]
                for b in range(4):
                    V("tensor_tensor", dict(out=pre[h][:, b + 1:b + 2], in0=pre[h][:, b:b + 1],
                                            in1=LF[:, h, b:b + 1], op=ALU.add),
                      r=[lfB, padB[h]], w=[padB[h]])
                yield
                P.mm(pc[:, 0:4], U[:], LF[:, h, :], True, False, reads=[lfB, cB], writes=[pcB_])
                P.mm(pc[:, 0:4], ones[:], pre[h][:, 0:4], False, True, reads=[padB[h], cB], writes=[pcB_])
                P.mm(pc[:, 4:5], ones[:], pre[h][:, 4:5], True, True, reads=[padB[h], cB], writes=[pcB_])
                yield
                V("tensor_scalar", dict(out=Ccol[:, h, 4 * i:4 * i + 4], in0=pc[:, 0:4],
                                        scalar1=carry[:, h:h + 1], scalar2=None, op0=ALU.add),
                  r=[pcB_], w=[ccB[h]])
                V("tensor_scalar", dict(out=biasT[p][h][:, 0:nkb], in0=Ccol[:, h, 0:nkb],
                                        scalar1=carry[:, h:h + 1], scalar2=-1.0,
                                        op0=ALU.subtract, op1=ALU.mult),
                  r=[ccB[h]], w=[biB[p][h]])
                V("tensor_copy", dict(out=pad[h][:], in_=pc[:, 0:4]),
                  r=[pcB_], w=[padB[h]])
                V("tensor_tensor", dict(out=carry[:, h:h + 1], in0=carry[:, h:h + 1],
                                        in1=pc[:, 4:5], op=ALU.add),
                  r=[pcB_, biB[p][h]], w=[ccB[h]])
                yield
                pa, paB = nextL()
                for b in range(4):
                    P.mm(pa[0:1, b * 128:(b + 1) * 128], pad[h][:, b:b + 1], idb[:], True, True,
                         reads=[padB[h], cB], writes=[paB])
                yield
                A("copy", dict(out=drow[p][h][0:1, :], in_=pa[0:1, :]), r=[paB], w=[drB[p][h]])
                yield

        def gen_S(i):
            p = i % 2
            cols = slice(i * 512, (i + 1) * 512)
            for b in range(4):
                k = b % 2
                bc = slice(b * 128, (b + 1) * 128)
                P.opk("tensor", "transpose", dict(out=tp[:, 0:128], in_=xcv[p][0][:, bc], identity=idb[:]),
                      [xcB[p][0], cB], [tpB])
                P.opk("tensor", "transpose", dict(out=tp[:, 128:256], in_=xcv[p][1][:, bc], identity=idb[:]),
                      [xcB[p][1], cB], [tpB])
                P.mm(pcb, xcv[p][1][:, bc], xcv[p][2][:, bc], True, True, reads=[xcB[p][1], xcB[p][2]],
                     writes=[pcbB])
                for h in range(2):
                    G("tensor_scalar", dict(
                        out=Wa[k][h][:, 0:128], in0=SL[:], scalar1=acol[p][:, b, h:h + 1], scalar2=None,
                        op0=ALU.mult), r=[colB[p], cB], w=[WaB[k][h]])
                    G("tensor_scalar", dict(
                        out=Wa[k][h][:, 128:256], in0=ones[:], scalar1=acol[p][:, b, h:h + 1], scalar2=None,
                        op0=ALU.mult), r=[colB[p], cB], w=[WaB[k][h]])
                yield
                A("copy", dict(out=xs_tok[k][:], in_=tp[:, 0:128]), r=[tpB], w=[tkB[k]])
                for h in range(2):
                    hc = slice(h * 64, (h + 1) * 64)
                    V("tensor_scalar", dict(
                        out=xdt[k][:, hc], in0=tp[:, hc], scalar1=dtc[p][:, b, h:h + 1], scalar2=None,
                        op0=ALU.mult), r=[tpB, colB[p]], w=[tkB[k]])
                A("copy", dict(out=Btok[k][:], in_=tp[:, 128:256]), r=[tpB], w=[tkB[k]])
                V("tensor_tensor", dict(out=cbm[k][:], in0=pcb, in1=U[:], op=ALU.mult),
                  r=[pcbB, cB], w=[cbmB[k]])
                for h in range(2):
                    P.mm(pseg[h][:, 0:128], Wa[k][h][:, 0:128], U[:], True, True, reads=[WaB[k][h], cB],
                         writes=[psegB[h]])
                    P.mm(pseg[h][:, 128:256], Wa[k][h][:, 128:256], U[:], True, True, reads=[WaB[k][h], cB],
                         writes=[psegB[h]])
                yield
                for h in range(2):
                    A("activation", dict(out=E[k][h][:], in_=pseg[h][:], func=AF.Exp),
                      r=[psegB[h]], w=[EB[k][h]])
                yield
                for h in range(2):
                    hc = slice(h * 64, (h + 1) * 64)
                    V("tensor_tensor", dict(out=MT[k][h][:], in0=E[k][h][:, 0:128], in1=cbm[k][:],
                                            op=ALU.mult), r=[EB[k][h], cbmB[k]], w=[mB[k][h]])
                    G("tensor_tensor", dict(out=Cp[k][h][:], in0=xcv[p][2][:, bc],
                                            in1=E[k][h][:, 128:256], op=ALU.mult),
                      r=[EB[k][h], xcB[p][2]], w=[mB[k][h]])
                    G("tensor_scalar", dict(out=xdtd[k][h][:], in0=xdt[k][:, hc],
                                            scalar1=E[k][h][:, 127:128], scalar2=None,
                                            op0=ALU.mult),
                      r=[EB[k][h], tkB[k]], w=[mB[k][h]])
                yield
                for h in range(2):
                    hc = slice(h * 64, (h + 1) * 64)
                    P.mm(py[:, hc], MT[k][h][:], xdt[k][:, hc], True, False, reads=[mB[k][h], tkB[k]],
                         writes=[pyB])
                    P.mm(py[:, hc], Cp[k][h][:], hTb[h][:], False, True, reads=[mB[k][h], hbB[h]], writes=[pyB])
                    P.mm(pst[:, hc], Btok[k][:], xdtd[k][h][:], True, True, reads=[tkB[k], mB[k][h]],
                         writes=[pstB])
                yield
                for h in range(2):
                    hc = slice(h * 64, (h + 1) * 64)
                    V("scalar_tensor_tensor", dict(
                        out=hTf[h][:], in0=hTf[h][:], scalar=E[k][h][:, 255:256], in1=pst[:, hc],
                        op0=ALU.mult, op1=ALU.add), r=[pstB, EB[k][h]], w=[hfB[h]])
                    V("tensor_copy", dict(out=hTb[h][:], in_=hTf[h][:]), r=[hfB[h]], w=[hbB[h]])
                    V("scalar_tensor_tensor", dict(
                        out=ytok[k][:, hc], in0=xs_tok[k][:, hc], scalar=hp[:, 4 + h:5 + h], in1=py[:, hc],
                        op0=ALU.mult, op1=ALU.add), r=[pyB, tkB[k], cB], w=[ytB[k]])
                yield
                P.opk("tensor", "transpose", dict(out=pyt, in_=ytok[k][:], identity=idf[:]),
                      [ytB[k], cB], [pytB])
                yield
                V("tensor_tensor", dict(out=ygt[:, bc], in0=pyt, in1=zs[p][:, bc],
                                        op=ALU.mult), r=[pytB, zsB[p]], w=[ygB])
                yield
            P.dma("sync", yT[0:128, cols], ygt[:], reads=[ygB])
            yield

        def gen_F(i):
            p = i % 2
            cols = slice(i * 512, (i + 1) * 512)
            nkb = 4 * i + 4
            for h in range(2):
                last = nkb - 1
                hp0 = 64 * h
                pend = []
                LAG = 2
                for kb in range(nkb + LAG):
                    if kb < nkb:
                        j = kb - 4 * i
                        q0 = 0 if j < 0 else j * 128
                        N = 512 - q0
                        ps, psB = nextF()
                        kk = cnt["pt"] % 3
                        cnt["pt"] += 1
                        P.mm(ps[:, 0:N], k2[:, kb * 128:(kb + 1) * 128], q2[p][h][:, q0:512],
                             True, False, reads=[kB_[kb // 4], q2B[p]], writes=[psB])
                        P.mm(ps[:, 0:N], onesb[:], drow[p][h][:, q0:512], False, True,
                             reads=[cB, drB[p][h]], writes=[psB])
                        A("activation", dict(
                            out=ptile[kk][:, 0:N], in_=ps[:, 0:N], func=AF.Exp, bias=biasT[p][h][:, kb:kb + 1]),
                          r=[psB, biB[p][h]], w=[ptB[kk]])
                        if j >= 0:
                            G("affine_select", dict(
                                out=ptile[kk][:, 0:128], in_=ptile[kk][:, 0:128], pattern=[[1, 128]],
                                compare_op=ALU.is_ge, fill=0.0, base=0, channel_multiplier=-1),
                              r=[ptB[kk]], w=[ptB[kk]])
                        pend.append((kb, q0, N, kk))
                    if kb >= LAG or kb >= nkb:
                        if pend and (kb >= nkb or len(pend) > LAG):
                            (pkb, pq0, pN, pkk) = pend.pop(0)
                            P.mm(po[0:65, pq0:512], Vaug[:, pkb, h, 0:65], ptile[pkk][:, 0:pN], pkb == 0, pkb == last,
                                 reads=[vB_[pkb // 4], ptB[pkk]], writes=[poB])
                    yield
                while pend:
                    (pkb, pq0, pN, pkk) = pend.pop(0)
                    P.mm(po[0:65, pq0:512], Vaug[:, pkb, h, 0:65], ptile[pkk][:, 0:pN], pkb == 0, pkb == last,
                         reads=[vB_[pkb // 4], ptB[pkk]], writes=[poB])
                yield
                A("copy", dict(out=oT[:], in_=po[0:65, :]), r=[poB], w=[oTB])
                yield
                P.mm(po[0:64, :], ones[64:65, 0:64], oT[64:65, :], True, True, reads=[oTB, cB], writes=[poB])
                yield
                V("reciprocal", dict(out=rec[:], in_=po[0:64, :]), r=[poB], w=[recB])
                V("tensor_tensor", dict(out=yfx[:], in0=oT[0:64, :], in1=rec[:], op=ALU.mult),
                  r=[oTB, recB], w=[yfxB])
                P.dma("sync", yT[128 + 64 * h:192 + 64 * h, cols], yfx[:], reads=[yfxB])
                yield

        def run_all(g):
            for _ in g:
                pass

        def merge(gens, weights):
            live = list(gens)
            wts = list(weights)
            while live:
                for idx in range(len(live) - 1, -1, -1):
                    pass
                nxt_live, nxt_w = [], []
                for g, wgt in zip(live, wts):
                    done = False
                    for _ in range(wgt):
                        try:
                            next(g)
                        except StopIteration:
                            done = True
                            break
                    if not done:
                        nxt_live.append(g)
                        nxt_w.append(wgt)
                live, wts = nxt_live, nxt_w

        run_all(gen_L(0))
        for i in range(NT):
            gens = [gen_F(i), gen_S(i)]
            wts = [max(1, (8 * i + 16) // 34), 1]
            if i + 1 < NT:
                gens.append(gen_L(i + 1))
                wts.append(1)
            merge(gens, wts)
        P.finish()
        P.emit()
    return nc


NEG = -1.0e30


def build_p2(TT, dbg=False, stages=(1, 2, 3, 4)):
    NTL = TT // 512
    NBL = TT // 128
    nc = bass.Bass("TRN2", target_bir_lowering=False)
    dram = lambda n, s, dt=F32, kind="ExternalInput": nc.dram_tensor(n, s, dt, kind=kind).ap()
    yTd = dram("yTin", [D, TT])
    xTd = dram("xTin", [D, TT])
    wod = dram("wo", [D, D])
    wqd = dram("wq", [D, D])
    snwd = dram("snw", [128, 8])
    ln2d = dram("ln2", [128, NCH])
    lnfd = dram("lnf", [128, NCH])
    k1Td = dram("k1T", [128, 8, 128])
    k2Td = dram("k2T", [128, 8, 128])
    uTd = dram("uT", [128, 128, D])
    vd = dram("vv", [16384, D])
    outT = dram("outT", [D, TT], kind="ExternalOutput")
    x1s = dram("x1s", [D, TT], kind="ExternalOutput" if dbg else "Internal")
    h2s = dram("h2s", [D, TT], BF16, kind="Internal")
    Sd = dram("Sd", [TT, 2048], kind="Internal")
    Gd = dram("Gd", [128, 128, TT], BF16, kind="ExternalOutput" if dbg else "Internal")
    v3 = lambda a: a.rearrange("(c p) t -> p c t", p=128)
    yTv, xTv, x1v, h2v, outv = v3(yTd), v3(xTd), v3(x1s), v3(h2s), v3(outT)
    wov = wod.rearrange("(c p) n -> p c n", p=128)
    wqv = wqd.rearrange("(c p) n -> p c n", p=128)
    ubd = dram("ubd", [128, 128, D], BF16, kind="Internal")
    vbd = dram("vbd", [128, 128, D], BF16, kind="Internal")

    with ExitStack() as es0:
        P = Prog(nc, es0)
        V = lambda f, kw, r=(), w=(): P.opk("vector", f, kw, r, w)
        G = lambda f, kw, r=(), w=(): P.opk("gpsimd", f, kw, r, w)
        A = lambda f, kw, r=(), w=(): P.opk("scalar", f, kw, r, w)

        stg = [0]

        def mk(es):
            stg[0] += 1
            pre = f"s{stg[0]}_"
            sb = lambda name, shape, dt=F32: es.enter_context(nc.sbuf_tensor(pre + name, shape, dt))
            pt = lambda name, shape, dt=F32: es.enter_context(nc.psum_tensor(pre + name, shape, dt))
            return sb, pt

        sb0, _ = mk(es0)
        cB = Buf("const")
        onesb = sb0("onesb", [128, 128], BF16)
        idb = sb0("idb", [128, 128], BF16)
        idf = sb0("idf", [128, 128])
        ln2t = sb0("ln2t", [128, NCH])
        lnft = sb0("lnft", [128, NCH])
        snwt = sb0("snwt", [128, 8])
        G("memset", dict(ap=onesb[:], constant=1.0), w=[cB])
        G("memset", dict(ap=idf[:], constant=1.0), w=[cB])
        G("affine_select", dict(out=idf[:], in_=idf[:], pattern=[[1, 128]], compare_op=ALU.is_equal,
                                fill=0.0, base=0, channel_multiplier=-1), w=[cB])
        G("tensor_copy", dict(out=idb[:], in_=idf[:]), r=[cB], w=[cB])
        P.dma("sync", ln2t[:], ln2d, writes=[cB])
        P.dma("sync", lnft[:], lnfd, writes=[cB])
        P.dma("sync", snwt[:], snwd, writes=[cB])

        def load_weight(sb, Wt, wB, wv, scale_t, nscaled, name):
            wst = [sb(f"{name}st{k}", [128, D]) for k in range(2)]
            wstB = [Buf(), Buf()]
            for c in range(NCH):
                k = c % 2
                P.dma("sync", wst[k][:], wv[:, c, :], writes=[wstB[k]])
                if c < nscaled:
                    V("tensor_scalar", dict(out=Wt[:, c, :], in0=wst[k][:], scalar1=scale_t[:, c:c + 1],
                                            scalar2=None, op0=ALU.mult), r=[wstB[k], cB], w=[wB])
                else:
                    A("copy", dict(out=Wt[:, c, :], in_=wst[k][:]), r=[wstB[k]], w=[wB])

        def rms_bcast(pss, pssB, src_chunks, srcB, sq, sqB, n, width):
            for ci, src in enumerate(src_chunks):
                k = ci % 2
                A("activation", dict(out=sq[k][:, 0:width], in_=src, func=AF.Square), r=[srcB], w=[sqB[k]])
                P.mm(pss[:, 0:width], onesb[:], sq[k][:, 0:width], ci == 0, ci == n - 1,
                     reads=[sqB[k], cB], writes=[pssB])

        if 1 in stages:
            with ExitStack() as es:
                sb, pt = mk(es)
                Wo = sb("Wo", [128, NCH, D], BF16)
                woB = Buf()
                load_weight(sb, Wo, woB, wov, snwt, 8, "wo")
                yt = sb("yt", [128, 4, 512])
                ytB = Buf()
                ynT = sb("ynT", [128, NCH, 512], BF16)
                ynB = Buf()
                sq = [sb(f"sq{k}", [128, 512], BF16) for k in range(2)]
                sqB = [Buf(), Buf()]
                rs = sb("rs", [128, 512])
                rsB = Buf()
                xc = [sb(f"xc{k}", [128, 512]) for k in range(2)]
                xcB = [Buf(), Buf()]
                x1T = sb("x1T", [128, NCH, 512])
                x1B = Buf()
                h2t = sb("h2t", [128, NCH, 512], BF16)
                h2B = Buf()
                acc = [pt(f"acc{k}", [128, 512]) for k in range(4)]
                accB = [Buf() for k in range(4)]
                an = [0]

                def nacc():
                    k = an[0] % 4
                    an[0] += 1
                    return acc[k], accB[k]
                for j in range(NTL):
                    cols = slice(j * 512, (j + 1) * 512)
                    for g4 in range(4):
                        P.dma("sync", yt[:], yTv[:, 4 * g4:4 * g4 + 4, cols], writes=[ytB])
                        if g4 < 2:
                            pss, pssB = nacc()
                            rms_bcast(pss, pssB, [yt[:, c, :] for c in range(4)], ytB, sq, sqB, 4, 512)
                            A("activation", dict(out=rs[:], in_=pss[:], func=AF.Ln, scale=1.0 / 512, bias=1e-6),
                              r=[pssB], w=[rsB])
                            A("activation", dict(out=rs[:], in_=rs[:], func=AF.Exp, scale=-0.5), r=[rsB], w=[rsB])
                            for c in range(4):
                                V("tensor_tensor", dict(out=ynT[:, 4 * g4 + c, :], in0=yt[:, c, :], in1=rs[:],
                                                        op=ALU.mult), r=[ytB, rsB], w=[ynB])
                        else:
                            for c in range(4):
                                V("tensor_copy", dict(out=ynT[:, 4 * g4 + c, :], in_=yt[:, c, :]), r=[ytB], w=[ynB])
                    for dc in range(NCH):
                        k = dc % 2
                        P.dma("sync", xc[k][:], xTv[:, dc, cols], writes=[xcB[k]])
                        ps, psB = nacc()
                        for c in range(NCH):
                            P.mm(ps[:], Wo[:, c, dc * 128:(dc + 1) * 128], ynT[:, c, :], c == 0, c == NCH - 1,
                                 reads=[woB, ynB], writes=[psB])
                        V("tensor_tensor", dict(out=x1T[:, dc, :], in0=ps[:], in1=xc[k][:], op=ALU.add),
                          r=[psB, xcB[k]], w=[x1B])
                    P.dma("sync", x1v[:, :, cols], x1T[:], reads=[x1B])
                    pss, pssB = nacc()
                    rms_bcast(pss, pssB, [x1T[:, c, :] for c in range(NCH)], x1B, sq, sqB, NCH, 512)
                    A("activation", dict(out=rs[:], in_=pss[:], func=AF.Ln, scale=1.0 / D, bias=1e-6),
                      r=[pssB], w=[rsB])
                    A("activation", dict(out=rs[:], in_=rs[:], func=AF.Exp, scale=-0.5), r=[rsB], w=[rsB])
                    for c in range(NCH):
                        V("tensor_tensor", dict(out=h2t[:, c, :], in0=x1T[:, c, :], in1=rs[:], op=ALU.mult),
                          r=[x1B, rsB], w=[h2B])
                    P.dma("sync", h2v[:, :, cols], h2t[:], reads=[h2B])
                P.barrier()
                P.emit()

        if 2 in stages:
            with ExitStack() as es:
                sb, pt = mk(es)
                Wq = sb("Wq", [128, NCH, D], BF16)
                wqB = Buf()
                load_weight(sb, Wq, wqB, wqv, ln2t, NCH, "wq")
                kst = sb("kst", [128, 8, 128])
                kT = [sb(f"kT{i}", [128, 8, 128], BF16) for i in range(2)]
                kB = Buf()
                for i, kd in enumerate((k1Td, k2Td)):
                    P.dma("sync", kst[:], kd, writes=[kB])
                    V("tensor_copy", dict(out=kT[i][:], in_=kst[:]), r=[kB], w=[kB])
                h2t = sb("h2t", [128, NCH, 512], BF16)
                h2B = Buf()
                qpT = sb("qpT", [128, NCH, 512], BF16)
                qpB = Buf()
                sall = [sb(f"sall{k}", [128, 2048]) for k in range(2)]
                saB = [Buf(), Buf()]
                acc = [pt(f"acc{k}", [128, 512]) for k in range(4)]
                accB = [Buf() for k in range(4)]
                an = [0]

                def nacc():
                    k = an[0] % 4
                    an[0] += 1
                    return acc[k], accB[k]
                for j in range(NTL):
                    cols = slice(j * 512, (j + 1) * 512)
                    P.dma("sync", h2t[:], h2v[:, :, cols], writes=[h2B])
                    for cc in range(NCH):
                        ps, psB = nacc()
                        for c in range(NCH):
                            P.mm(ps[:], Wq[:, c, cc * 128:(cc + 1) * 128], h2t[:, c, :], c == 0, c == NCH - 1,
                                 reads=[wqB, h2B], writes=[psB])
                        if cc % 2 == 0:
                            A("copy", dict(out=qpT[:, cc, :], in_=ps[:]), r=[psB], w=[qpB])
                        else:
                            V("tensor_copy", dict(out=qpT[:, cc, :], in_=ps[:]), r=[psB], w=[qpB])
                    for b in range(4):
                        k = b % 2
                        bc = slice(b * 128, (b + 1) * 128)
                        for q4 in range(4):
                            ps, psB = nacc()
                            for c4 in range(4):
                                cc = 4 * q4 + c4
                                P.mm(ps[:, c4 * 128:(c4 + 1) * 128], qpT[:, cc, bc], kT[cc % 2][:, cc // 2, :],
                                     True, True, reads=[qpB, kB], writes=[psB])
                            if q4 % 2 == 0:
                                A("copy", dict(out=sall[k][:, q4 * 512:(q4 + 1) * 512], in_=ps[:]), r=[psB], w=[saB[k]])
                            else:
                                V("tensor_copy", dict(out=sall[k][:, q4 * 512:(q4 + 1) * 512], in_=ps[:]),
                                  r=[psB], w=[saB[k]])
                        t0 = j * 512 + b * 128
                        P.dma("sync", Sd[t0:t0 + 128, :], sall[k][:], reads=[saB[k]])
                P.barrier()
                P.emit()

        if 3 in stages:
            with ExitStack() as es:
                sb, pt = mk(es)
                sall = [sb(f"sall{p}", [128, 8, 2, 128]) for p in range(2)]
                saB = [Buf(), Buf()]
                top = [sb(f"top{p}", [128, 8, 2, 16]) for p in range(2)]
                topB = [Buf(), Buf()]
                sm8 = [sb(f"sm8{p}", [128, 8 * 8]) for p in range(2)]
                rB = [Buf(), Buf()]
                swork = sb("swork", [128, 128])
                cand = sb("cand", [128, 8, 256])
                cwork = sb("cwork", [128, 256])
                c16 = sb("c16", [128, 8, 16])
                cdB = Buf()
                smm = [sb(f"smm{k}", [128, 16, 128]) for k in range(2)]
                smB = [Buf(), Buf()]
                msk = [sb(f"msk{k}", [128, 16, 128], BF16) for k in range(2)]
                mkB = [Buf(), Buf()]
                ex = [sb(f"ex{k}", [128, 16, 128]) for k in range(2)]
                exB = [Buf(), Buf()]
                R = sb("R", [128, 128, 128], BF16)
                RB = Buf()
                OH = sb("OH", [128, 128, 128], BF16)
                OHB = Buf()
                RT = sb("RT", [128, 128, 64], BF16)
                RTB = Buf()
                PT = sb("PT", [128, 128, 64], BF16)
                PTB = Buf()
                Gs2 = [sb(f"Gs{k}", [128, 128, 64], BF16) for k in range(2)]
                Gs2B = [Buf(), Buf()]
                tpp = [pt(f"tpp{k}", [128, 1024], BF16) for k in range(3)]
                tpB = [Buf() for k in range(3)]
                pg = [pt(f"pg{k}", [128, 512]) for k in range(3)]
                pgB = [Buf() for k in range(3)]
                rr = {"tp": 0, "pg": 0}

                def gen_X(blk):
                    p = blk % 2
                    t0 = blk * 128
                    thr, mx, negm, Z, lnZ, adj = [sm8[p][:, 8 * i:8 * i + 8] for i in range(6)]
                    P.dma("sync", sall[p][:], Sd[t0:t0 + 128, :].rearrange("t (h f n) -> t h f n", h=8, f=2),
                          writes=[saB[p]])
                    for h in range(8):
                        for f in range(2):
                            V("max", dict(out=top[p][:, h, f, 0:8], in_=sall[p][:, h, f, :]), r=[saB[p]], w=[topB[p]])
                            V("match_replace", dict(out=swork[:], in_to_replace=top[p][:, h, f, 0:8],
                                                    in_values=sall[p][:, h, f, :], imm_value=NEG),
                              r=[saB[p], topB[p]], w=[cdB])
                            V("max", dict(out=top[p][:, h, f, 8:16], in_=swork[:]), r=[cdB], w=[topB[p]])
                        yield
                    V("tensor_tensor", dict(out=cand[:].rearrange("p h (a b) -> p h a b", a=16),
                                            in0=top[p][:, :, 0, :].unsqueeze(3).to_broadcast([128, 8, 16, 16]),
                                            in1=top[p][:, :, 1, :].unsqueeze(2).to_broadcast([128, 8, 16, 16]),
                                            op=ALU.add), r=[topB[p]], w=[cdB])
                    for h in range(8):
                        V("max", dict(out=c16[:, h, 0:8], in_=cand[:, h, :]), r=[cdB], w=[cdB])
                        V("match_replace", dict(out=cwork[:], in_to_replace=c16[:, h, 0:8], in_values=cand[:, h, :],
                                                imm_value=NEG), r=[cdB], w=[cdB])
                        V("max", dict(out=c16[:, h, 8:16], in_=cwork[:]), r=[cdB], w=[cdB])
                        if h % 2 == 1:
                            yield
                    V("tensor_reduce", dict(out=thr, in_=c16[:], axis=AX.X, op=ALU.min), r=[cdB], w=[rB[p]])
                    V("tensor_reduce", dict(out=mx, in_=c16[:], axis=AX.X, op=ALU.max), r=[cdB], w=[rB[p]])
                    V("tensor_scalar", dict(out=negm, in0=mx, scalar1=-1.0, scalar2=None, op0=ALU.mult),
                      r=[rB[p]], w=[rB[p]])
                    V("tensor_tensor", dict(out=c16[:], in0=c16[:], in1=mx.unsqueeze(2).to_broadcast([128, 8, 16]),
                                            op=ALU.subtract), r=[cdB, rB[p]], w=[cdB])
                    A("activation", dict(out=c16[:], in_=c16[:], func=AF.Exp), r=[cdB], w=[cdB])
                    V("tensor_reduce", dict(out=Z, in_=c16[:], axis=AX.X, op=ALU.add), r=[cdB], w=[rB[p]])
                    A("activation", dict(out=lnZ, in_=Z, func=AF.Ln), r=[rB[p]], w=[rB[p]])
                    V("tensor_tensor", dict(out=adj, in0=negm, in1=lnZ, op=ALU.subtract), r=[rB[p]], w=[rB[p]])
                    yield

                def gen_X2(blk):
                    p = blk % 2
                    thr, mx, negm, Z, lnZ, adj = [sm8[p][:, 8 * i:8 * i + 8] for i in range(6)]
                    for h in range(8):
                        k = h % 2
                        hs = slice(16 * h, 16 * h + 16)
                        v1b = top[p][:, h, 0, :].unsqueeze(2).to_broadcast([128, 16, 128])
                        V("tensor_tensor", dict(out=smm[k][:], in0=v1b,
                                                in1=sall[p][:, h, 1, :].unsqueeze(1).to_broadcast([128, 16, 128]),
                                                op=ALU.add), r=[topB[p], saB[p]], w=[smB[k]])
                        V("tensor_scalar", dict(out=msk[k][:], in0=smm[k][:], scalar1=thr[:, h:h + 1], scalar2=None,
                                                op0=ALU.is_ge), r=[smB[k], rB[p]], w=[mkB[k]])
                        A("activation", dict(out=ex[k][:], in_=smm[k][:], func=AF.Exp, bias=adj[:, h:h + 1]),
                          r=[smB[k], rB[p]], w=[exB[k]])
                        G("tensor_tensor", dict(out=R[:, hs, :], in0=ex[k][:], in1=msk[k][:], op=ALU.mult),
                          r=[exB[k], mkB[k]], w=[RB])
                        V("tensor_tensor", dict(out=OH[:, hs, :], in0=v1b,
                                                in1=sall[p][:, h, 0, :].unsqueeze(1).to_broadcast([128, 16, 128]),
                                                op=ALU.is_equal), r=[topB[p], saB[p]], w=[OHB])
                        yield

                def gen_Y(blk):
                    t0 = blk * 128
                    for th in range(2):
                        ts_ = slice(64 * th, 64 * th + 64)
                        Gs, GsB = Gs2[th], Gs2B[th]
                        pend = None
                        nev = 0
                        for (src, srcB, dst, dstB) in ((R, RB, RT, RTB), (OH, OHB, PT, PTB)):
                            for i8 in range(16):
                                kk = rr["tp"] % 3
                                rr["tp"] += 1
                                for ii in range(8):
                                    i = 8 * i8 + ii
                                    P.opk("tensor", "transpose", dict(out=tpp[kk][:, ii * 64:(ii + 1) * 64],
                                                                      in_=src[ts_, :, i], identity=idb[ts_, ts_]),
                                          [srcB, cB], [tpB[kk]])
                                if pend is not None:
                                    pend()
                                def ev(kk=kk, i8=i8, dst=dst, dstB=dstB, n=nev):
                                    f = A
                                    f("copy",
                                      dict(out=dst[:, 8 * i8:8 * i8 + 8, :],
                                           in_=tpp[kk][:, 0:512].rearrange("p (i t) -> p i t", t=64)),
                                      r=[tpB[kk]], w=[dstB])
                                pend = ev
                                nev += 1
                                yield
                        pend()
                        pend = None
                        for t4 in range(16):
                            kk = rr["pg"] % 3
                            rr["pg"] += 1
                            for tt in range(4):
                                t = 4 * t4 + tt
                                P.mm(pg[kk][:, tt * 128:(tt + 1) * 128], RT[:, :, t], PT[:, :, t], True, True,
                                     reads=[RTB, PTB], writes=[pgB[kk]])
                            if pend is not None:
                                pend()
                            def ev2(kk=kk, t4=t4, Gs=Gs, GsB=GsB):
                                dsto = Gs[:, :, 4 * t4:4 * t4 + 4].rearrange("p i t -> p t i")
                                srci = pg[kk][:].rearrange("p (t i) -> p t i", t=4)
                                A("copy", dict(out=dsto, in_=srci), r=[pgB[kk]], w=[GsB])
                            pend = ev2
                            yield
                        pend()
                        tg = t0 + 64 * th
                        for q in range(8):
                            P.dma("sync", Gd[16 * q:16 * q + 16, :, tg:tg + 64].rearrange("a b t -> b a t"),
                                  Gs[:, 16 * q:16 * q + 16, :], reads=[GsB])
                        yield

                def merge2(ga, gb, wa=1):
                    live = [(g, w) for g, w in ((ga, wa), (gb, 1)) if g is not None]
                    while live:
                        for (g, w) in list(live):
                            for _ in range(w):
                                try:
                                    next(g)
                                except StopIteration:
                                    live.remove((g, w))
                                    break

                merge2(gen_X(0), None)
                merge2(gen_X2(0), None)
                for blk in range(NBL):
                    merge2(gen_Y(blk), gen_X(blk + 1) if blk + 1 < NBL else None, wa=4)
                    if blk + 1 < NBL:
                        merge2(gen_X2(blk + 1), None)
                P.barrier()
                P.emit()

        if 4 in stages:
            with ExitStack() as es:
                sb, pt = mk(es)
                GE = 4
                h2t = sb("h2t", [128, NCH, 512], BF16)
                h2B = Buf()
                ust = [sb(f"ust{k}", [128, NCH, 128]) for k in range(2)]
                ustB = [Buf(), Buf()]
                ub = [sb(f"ub{k}", [128, NCH, 128], BF16) for k in range(3)]
                ubB = [Buf() for k in range(3)]
                vst = [sb(f"vst{k}", [128, D]) for k in range(2)]
                vstB = [Buf(), Buf()]
                vb = [sb(f"vb{k}", [128, D], BF16) for k in range(2 * GE)]
                vbB = [Buf() for k in range(2 * GE)]
                gch = [sb(f"gch{k}", [128, 512], BF16) for k in range(3)]
                gchB = [Buf() for k in range(3)]
                ge = [sb(f"ge{k}", [128, 512]) for k in range(2)]
                geB = [Buf(), Buf()]
                Wt = [sb(f"Wt{k}", [128, 512], BF16) for k in range(2 * GE)]
                WtB = [Buf() for k in range(2 * GE)]
                oacc = sb("oacc", [128, NCH, 512])
                oaB = Buf()
                xc = [sb(f"xc{k}", [128, 512]) for k in range(2)]
                xcB = [Buf(), Buf()]
                sq = [sb(f"sq{k}", [128, 512], BF16) for k in range(2)]
                sqB = [Buf(), Buf()]
                rs = sb("rs", [128, 512])
                rsB = Buf()
                ot = [sb(f"ot{k}", [128, 512]) for k in range(2)]
                otB = [Buf(), Buf()]
                pa = [pt(f"pa{k}", [128, 512]) for k in range(3)]
                paB = [Buf() for k in range(3)]
                pacc = [pt(f"pacc{k}", [128, 512]) for k in range(3)]
                paccB = [Buf() for k in range(3)]
                pn = [0, 0]
                ln2b = ln2t[:, :].unsqueeze(2).to_broadcast([128, NCH, 128])
                ubdB = [Buf() for _ in range(128)]
                vbdB = [Buf() for _ in range(128)]
                pend_grp = None

                def emit_group(base, first):
                    for dc in range(NCH):
                        kq = pn[1] % 3
                        pn[1] += 1
                        for gi in range(GE):
                            P.mm(pacc[kq][:], vb[base + gi][:, dc * 128:(dc + 1) * 128], Wt[base + gi][:],
                                 gi == 0, gi == GE - 1, reads=[vbB[base + gi], WtB[base + gi]],
                                 writes=[paccB[kq]])
                        if first:
                            V("tensor_copy", dict(out=oacc[:, dc, :], in_=pacc[kq][:]), r=[paccB[kq]], w=[oaB])
                        else:
                            V("tensor_tensor", dict(out=oacc[:, dc, :], in0=oacc[:, dc, :], in1=pacc[kq][:],
                                                    op=ALU.add), r=[paccB[kq]], w=[oaB])
                for j in range(NTL):
                    cols = slice(j * 512, (j + 1) * 512)
                    P.dma("sync", h2t[:], h2v[:, :, cols], writes=[h2B])
                    for i1 in range(128):
                        s2 = i1 % 2
                        s3 = i1 % 3
                        sv = i1 % (2 * GE)
                        es_ = slice(i1 * 128, (i1 + 1) * 128)
                        P.dma("sync", gch[s3][:], Gd[i1, :, cols], writes=[gchB[s3]])
                        if j == 0:
                            P.dma("sync", ust[s2][:].rearrange("p c e -> p (c e)"), uTd[i1, :, :], writes=[ustB[s2]])
                            P.dma("sync", vst[s2][:], vd[es_, :], writes=[vstB[s2]])
                            G("tensor_tensor", dict(out=ub[s3][:], in0=ust[s2][:], in1=ln2b, op=ALU.mult),
                              r=[ustB[s2], cB], w=[ubB[s3]])
                            if i1 % 2 == 0:
                                V("tensor_copy", dict(out=vb[sv][:], in_=vst[s2][:]), r=[vstB[s2]], w=[vbB[sv]])
                            else:
                                A("copy", dict(out=vb[sv][:], in_=vst[s2][:]), r=[vstB[s2]], w=[vbB[sv]])
                            if NTL > 1:
                                P.dma("scalar", ubd[i1, :, :], ub[s3][:].rearrange("p c e -> p (c e)"),
                                      reads=[ubB[s3]], writes=[ubdB[i1]])
                                P.dma("scalar", vbd[i1, :, :], vb[sv][:], reads=[vbB[sv]], writes=[vbdB[i1]])
                        else:
                            P.dma("sync", ub[s3][:].rearrange("p c e -> p (c e)"), ubd[i1, :, :],
                                  reads=[ubdB[i1]], writes=[ubB[s3]])
                            P.dma("sync", vb[sv][:], vbd[i1, :, :], reads=[vbdB[i1]], writes=[vbB[sv]])
                        kp = pn[0] % 3
                        pn[0] += 1
                        for c in range(NCH):
                            P.mm(pa[kp][:], ub[s3][:, c, :], h2t[:, c, :], c == 0, c == NCH - 1,
                                 reads=[ubB[s3], h2B], writes=[paB[kp]])
                        if pend_grp is not None:
                            emit_group(*pend_grp)
                            pend_grp = None
                        A("activation", dict(out=ge[s2][:], in_=pa[kp][:], func=AF.Gelu), r=[paB[kp]], w=[geB[s2]])
                        V("tensor_tensor", dict(out=Wt[sv][:], in0=ge[s2][:], in1=gch[s3][:], op=ALU.mult),
                          r=[geB[s2], gchB[s3]], w=[WtB[sv]])
                        if i1 % GE == GE - 1:
                            pend_grp = (sv - (GE - 1), i1 == GE - 1)
                    if pend_grp is not None:
                        emit_group(*pend_grp)
                        pend_grp = None
                    for dc in range(NCH):
                        k = dc % 2
                        P.dma("sync", xc[k][:], x1v[:, dc, cols], writes=[xcB[k]])
                        V("tensor_tensor", dict(out=oacc[:, dc, :], in0=oacc[:, dc, :], in1=xc[k][:], op=ALU.add),
                          r=[xcB[k]], w=[oaB])
                    kq = pn[1] % 3
                    pn[1] += 1
                    rms_bcast(pacc[kq], paccB[kq], [oacc[:, c, :] for c in range(NCH)], oaB, sq, sqB, NCH, 512)
                    A("activation", dict(out=rs[:], in_=pacc[kq][:], func=AF.Ln, scale=1.0 / D, bias=1e-6),
                      r=[paccB[kq]], w=[rsB])
                    A("activation", dict(out=rs[:], in_=rs[:], func=AF.Exp, scale=-0.5), r=[rsB], w=[rsB])
                    for dc in range(NCH):
                        k = dc % 2
                        V("scalar_tensor_tensor", dict(out=ot[k][:], in0=oacc[:, dc, :], scalar=lnft[:, dc:dc + 1],
                                                       in1=rs[:], op0=ALU.mult, op1=ALU.mult),
                          r=[oaB, rsB, cB], w=[otB[k]])
                        P.dma("sync", outv[:, dc, cols], ot[k][:], reads=[otB[k]])
                P.barrier()
                P.emit()
        P.finish()
        P.emit()
    return nc


def prep_p1(inp, c, T):
    g = c // 4
    w_in = inp["w_in"]
    cols = np.concatenate([
        np.arange(0 + 128 * c, 128 * c + 128),
        np.arange(1024 + 128 * c, 1024 + 128 * c + 128),
        np.arange(2048 + 128 * g, 2048 + 128 * g + 128),
        np.arange(2304 + 128 * g, 2304 + 128 * g + 128),
        np.arange(2576 + 128 * c, 2576 + 128 * c + 128),
        np.arange(3600 + 128 * c, 3600 + 128 * c + 128),
        np.arange(4624 + 128 * c, 4624 + 128 * c + 128),
        np.arange(2560 + 2 * c, 2560 + 2 * c + 2),
        np.arange(5648 + 2 * c, 5648 + 2 * c + 2),
    ])
    wf = np.ascontiguousarray(w_in[:, cols])
    ln1 = np.ascontiguousarray(inp["ln1_w"].reshape(16, 128).T)
    chans = [np.arange(128 * c, 128 * c + 128), np.arange(1024 + 128 * g, 1024 + 128 * g + 128),
             np.arange(1280 + 128 * g, 1280 + 128 * g + 128)]
    cw = np.zeros((128, 12), np.float32)
    cb = np.zeros((128, 3), np.float32)
    for gi, ch in enumerate(chans):
        cw[:, 4 * gi:4 * gi + 4] = inp["conv_w"][:, ch].T
        cb[:, gi] = inp["conv_b"][ch]
    hp = np.zeros((128, 8), np.float32)
    for k, name in enumerate(["dt_bias", "a_log", "d_skip", "fox_f_bias"]):
        hp[:, 2 * k] = inp[name][2 * c]
        hp[:, 2 * k + 1] = inp[name][2 * c + 1]
    return {"wf": wf, "ln1": ln1, "cw": cw, "cb": cb, "hp": hp}


SEQ = 16384
NCORES = 8


def kernel(x, ln1_w, w_in, conv_w, conv_b, dt_bias, a_log, d_skip, ssd_norm_w,
           fox_f_bias, w_out, ln2_w, peer_wq, peer_k1, peer_k2, peer_u, peer_v, lnf_w):
    f32 = lambda a: np.ascontiguousarray(np.asarray(a, dtype=np.float32))
    inp = {"w_in": f32(w_in), "ln1_w": f32(ln1_w), "conv_w": f32(conv_w), "conv_b": f32(conv_b),
           "dt_bias": f32(dt_bias), "a_log": f32(a_log), "d_skip": f32(d_skip), "fox_f_bias": f32(fox_f_bias)}
    T = SEQ
    xT = np.ascontiguousarray(f32(x)[0].T)
    nc1 = build_p1(T)
    in_maps = []
    for c in range(NCORES):
        d = prep_p1(inp, c, T)
        d["xT"] = xT
        in_maps.append(d)
    res1 = run_bass_kernel_spmd(nc1, in_maps, core_ids=list(range(NCORES)))
    yT = np.empty((2048, T), np.float32)
    for c in range(NCORES):
        r = res1.results[c]["yT"]
        yT[128 * c:128 * c + 128] = r[0:128]
        yT[1024 + 128 * c:1024 + 128 * c + 128] = r[128:256]
    TT = T // NCORES
    nc2 = build_p2(TT)
    cl = lambda a: np.ascontiguousarray(f32(a).reshape(16, 128).T)
    shared = {"wo": f32(w_out), "wq": f32(peer_wq),
              "snw": np.ascontiguousarray(f32(ssd_norm_w).reshape(8, 128).T), "ln2": cl(ln2_w), "lnf": cl(lnf_w),
              "k1T": np.ascontiguousarray(f32(peer_k1).transpose(2, 0, 1)),
              "k2T": np.ascontiguousarray(f32(peer_k2).transpose(2, 0, 1)),
              "uT": np.ascontiguousarray(f32(peer_u).reshape(128, 128, 16, 128).transpose(0, 3, 2, 1)).reshape(128, 128, 2048),
              "vv": f32(peer_v)}
    in_maps = []
    for c in range(NCORES):
        d = dict(shared)
        d["yTin"] = np.ascontiguousarray(yT[:, c * TT:(c + 1) * TT])
        d["xTin"] = np.ascontiguousarray(xT[:, c * TT:(c + 1) * TT])
        in_maps.append(d)
    res2 = run_bass_kernel_spmd(nc2, in_maps, core_ids=list(range(NCORES)))
    out = np.empty((1, T, 2048), np.float32)
    for c in range(NCORES):
        out[0, c * TT:(c + 1) * TT, :] = res2.results[c]["outT"].T
    return out
```
